# Optimizing a Trainium2 kernel written in Bass

```python
import math
import jax, jax.numpy as jnp
from jax import lax
import numpy as np

D_MODEL = 1024
BATCH = 8
SEQ = 2048
DEPTH = 2

GRID_W = 64
CTX_LEN = 256

ATTN_HEADS = 4
ATTN_DIM = 64
ATTN_VDIM = 2 * ATTN_DIM
Q_W = ATTN_HEADS * 2 * ATTN_DIM
K_W = ATTN_HEADS * 2 * ATTN_DIM
V_W = ATTN_HEADS * ATTN_VDIM
CONV_WIDTH = D_MODEL - V_W
CONV_K = 3
IN_EVEN = Q_W + K_W + V_W + 3 * CONV_WIDTH
Q_BLOCK = 128
ROPE_BASE = 10000.0

SGU_WIDTH = D_MODEL
SGU_GROUPS = 4
CHUNK = 128

D_FF = 2816
N_EXPERTS = 8
TOP_K = 2
D_FF_EXPERT = 3584

N_EVEN = (DEPTH + 1) // 2
N_ODD = DEPTH // 2
EPS = 1e-6

kernel_name = "hybrid_diffattn_shortconv_gmlp_moe_dit"


def rmsnorm(x, g):
    xf = x.astype(jnp.float32)
    y = xf * lax.rsqrt(jnp.mean(xf * xf, axis=-1, keepdims=True) + EPS)
    return (y * g.astype(jnp.float32)).astype(x.dtype)


def layernorm(x, g, b):
    xf = x.astype(jnp.float32)
    mu = jnp.mean(xf, axis=-1, keepdims=True)
    var = jnp.mean(jnp.square(xf - mu), axis=-1, keepdims=True)
    y = (xf - mu) * lax.rsqrt(var + EPS)
    return (y * g.astype(jnp.float32) + b.astype(jnp.float32)).astype(x.dtype)


def modulate(x, g, shift, scale):
    return rmsnorm(x, g) * (1 + scale) + shift


def ada_chunks(cond, w, b):
    m = jax.nn.silu(cond) @ w + b
    return jnp.split(m, 6, axis=-1)


def axial_rope_tables(rows, dim):
    row = jnp.repeat(jnp.arange(rows), GRID_W).astype(jnp.float32)
    col = jnp.tile(jnp.arange(GRID_W), rows).astype(jnp.float32)
    quarter = dim // 4
    inv = ROPE_BASE ** (-jnp.arange(quarter, dtype=jnp.float32) / quarter)
    ar = row[:, None] * inv
    ac = col[:, None] * inv
    ang = jnp.concatenate([ar, ar, ac, ac], axis=-1)
    return jnp.cos(ang), jnp.sin(ang)


def apply_rope(t, cos, sin):
    cos = cos[:, None, None, :].astype(t.dtype)
    sin = sin[:, None, None, :].astype(t.dtype)
    t1, t2, t3, t4 = jnp.split(t, 4, axis=-1)
    rot = jnp.concatenate([-t2, t1, -t4, t3], axis=-1)
    return t * cos + rot * sin


def diff_attention(q, k, v, lam):
    B, S, H, _, d = q.shape
    nb = S // Q_BLOCK
    qb = q.reshape(B, nb, Q_BLOCK, H, 2, d).swapaxes(0, 1)
    scale = d ** -0.5

    def block(qi):
        s = jnp.einsum('bqhcd,bkhcd->bhcqk', qi, k).astype(jnp.float32) * scale
        p = jax.nn.softmax(s, axis=-1)
        w = (p[:, :, 0] - lam * p[:, :, 1]).astype(v.dtype)
        return jnp.einsum('bhqk,bkhe->bqhe', w, v)

    out = lax.map(block, qb)
    return out.swapaxes(0, 1).reshape(B, S, H, v.shape[-1])


def short_conv(x, w):
    C = x.shape[-1]
    pad = CONV_K // 2
    return lax.conv_general_dilated(
        x, w[:, None, :].astype(x.dtype), window_strides=(1,), padding=((pad, pad),),
        dimension_numbers=('NWC', 'WIO', 'NWC'), feature_group_count=C)


def even_mixer(h, hc, w_in, q_g, k_g, lq1, lk1, lq2, lk2, subln_g, conv_w, w_out,
               cos, sin, lambda_init):
    B, S, _ = h.shape
    L = hc.shape[1]
    proj = h @ w_in
    o1 = Q_W
    o2 = o1 + K_W
    o3 = o2 + V_W
    o4 = o3 + CONV_WIDTH
    o5 = o4 + CONV_WIDTH
    q, k, v, gate_b, gate_c, u = jnp.split(proj, [o1, o2, o3, o4, o5], axis=-1)
    q = apply_rope(rmsnorm(q.reshape(B, S, ATTN_HEADS, 2, ATTN_DIM), q_g), cos, sin)
    k = apply_rope(rmsnorm(k.reshape(B, S, ATTN_HEADS, 2, ATTN_DIM), k_g), cos, sin)
    v = v.reshape(B, S, ATTN_HEADS, ATTN_VDIM)
    kc, vc = jnp.split(hc @ w_in[:, o1:o3], [K_W], axis=-1)
    kc = rmsnorm(kc.reshape(B, L, ATTN_HEADS, 2, ATTN_DIM), k_g)
    vc = vc.reshape(B, L, ATTN_HEADS, ATTN_VDIM)
    k_all = jnp.concatenate([kc, k], axis=1)
    v_all = jnp.concatenate([vc, v], axis=1)
    lam = (jnp.exp(jnp.sum(lq1.astype(jnp.float32) * lk1.astype(jnp.float32)))
           - jnp.exp(jnp.sum(lq2.astype(jnp.float32) * lk2.astype(jnp.float32)))
           + lambda_init)
    attn = diff_attention(q, k_all, v_all, lam)
    attn = rmsnorm(attn, subln_g) * (1.0 - lambda_init)
    y_conv = gate_b * short_conv(gate_c * u, conv_w)
    y = jnp.concatenate([attn.reshape(B, S, V_W), y_conv], axis=-1)
    return y @ w_out


def sgu_mixer(h, w_in, ln_g, ln_b, w_s, b_s, w_out):
    B, S, _ = h.shape
    z = jax.nn.gelu(h @ w_in)
    u, v = jnp.split(z, 2, axis=-1)
    v = layernorm(v, ln_g, ln_b)
    n = S // CHUNK
    vg = v.reshape(B, n, CHUNK, SGU_GROUPS, SGU_WIDTH // SGU_GROUPS)
    s = jnp.einsum('gpq,bnqgc->bnpgc', w_s, vg) + b_s.T[:, :, None]
    return (u * s.reshape(B, S, SGU_WIDTH)) @ w_out


def swiglu(h, wg, wu, wd):
    return (jax.nn.silu(h @ wg) * (h @ wu)) @ wd


def moe_swiglu(h, router_w, wg, wu, wd):
    logits = (h @ router_w).astype(jnp.float32)
    top_v, top_i = lax.top_k(logits, TOP_K)
    gates = jax.nn.softmax(top_v, axis=-1)
    dense_gate = jnp.sum(jax.nn.one_hot(top_i, N_EXPERTS, dtype=jnp.float32)
                         * gates[..., None], axis=-2).astype(h.dtype)
    out = jnp.zeros_like(h)
    for e in range(N_EXPERTS):
        out = out + dense_gate[..., e:e + 1] * swiglu(h, wg[e], wu[e], wd[e])
    return out


def setup_inputs(seed: int = 0) -> dict:
    key = jax.random.key(seed)
    ks = iter(jax.random.split(key, 40))
    D = D_MODEL

    def nrm(shape, scale):
        return jax.random.normal(next(ks), shape, jnp.float32) * scale

    def gain(shape):
        return 1.0 + 0.01 * jax.random.normal(next(ks), shape, jnp.float32)

    return {
        "x": nrm((BATCH, SEQ, D), 1.0),
        "c": nrm((BATCH, D), 1.0),
        "ctx": nrm((BATCH, CTX_LEN, D), 1.0),
        "c_ctx": nrm((D,), 1.0),
        "ada_w": nrm((DEPTH, D, 6 * D), D ** -0.5),
        "ada_b": nrm((DEPTH, 6 * D), 0.01),
        "norm_mix_g": gain((DEPTH, D)),
        "norm_ffn_g": gain((DEPTH, D)),
        "w_in_even": nrm((N_EVEN, D, IN_EVEN), D ** -0.5),
        "q_norm_g": gain((N_EVEN, ATTN_DIM)),
        "k_norm_g": gain((N_EVEN, ATTN_DIM)),
        "lam_q1": nrm((N_EVEN, ATTN_DIM), 0.1),
        "lam_k1": nrm((N_EVEN, ATTN_DIM), 0.1),
        "lam_q2": nrm((N_EVEN, ATTN_DIM), 0.1),
        "lam_k2": nrm((N_EVEN, ATTN_DIM), 0.1),
        "subln_g": gain((N_EVEN, ATTN_VDIM)),
        "conv_w": nrm((N_EVEN, CONV_K, CONV_WIDTH), CONV_K ** -0.5),
        "w_out_even": nrm((N_EVEN, V_W + CONV_WIDTH, D), (V_W + CONV_WIDTH) ** -0.5),
        "ffn_w_gate": nrm((N_EVEN, D, D_FF), D ** -0.5),
        "ffn_w_up": nrm((N_EVEN, D, D_FF), D ** -0.5),
        "ffn_w_down": nrm((N_EVEN, D_FF, D), D_FF ** -0.5),
        "sgu_w_in": nrm((N_ODD, D, 2 * SGU_WIDTH), D ** -0.5),
        "sgu_ln_g": gain((N_ODD, SGU_WIDTH)),
        "sgu_ln_b": nrm((N_ODD, SGU_WIDTH), 0.01),
        "sgu_w_s": nrm((N_ODD, SGU_GROUPS, CHUNK, CHUNK), CHUNK ** -0.5),
        "sgu_b_s": nrm((N_ODD, SGU_GROUPS, CHUNK), 0.01),
        "sgu_w_out": nrm((N_ODD, SGU_WIDTH, D), SGU_WIDTH ** -0.5),
        "router_w": nrm((N_ODD, D, N_EXPERTS), D ** -0.5),
        "moe_w_gate": nrm((N_ODD, N_EXPERTS, D, D_FF_EXPERT), D ** -0.5),
        "moe_w_up": nrm((N_ODD, N_EXPERTS, D, D_FF_EXPERT), D ** -0.5),
        "moe_w_down": nrm((N_ODD, N_EXPERTS, D_FF_EXPERT, D), D_FF_EXPERT ** -0.5),
    }


def reference(x, c, ctx, c_ctx, ada_w, ada_b, norm_mix_g, norm_ffn_g,
              w_in_even, q_norm_g, k_norm_g, lam_q1, lam_k1, lam_q2, lam_k2, subln_g,
              conv_w, w_out_even, ffn_w_gate, ffn_w_up, ffn_w_down,
              sgu_w_in, sgu_ln_g, sgu_ln_b, sgu_w_s, sgu_b_s, sgu_w_out,
              router_w, moe_w_gate, moe_w_up, moe_w_down):
    rows = x.shape[1] // GRID_W
    cos, sin = axial_rope_tables(rows, ATTN_DIM)
    h = x
    for i in range(DEPTH):
        sm, cm, gm, sf, cf, gf = [t[:, None, :] for t in ada_chunks(c, ada_w[i], ada_b[i])]
        hm = modulate(h, norm_mix_g[i], sm, cm)
        j = i // 2
        if i % 2 == 0:
            c_sm, c_cm, _, _, _, _ = ada_chunks(c_ctx, ada_w[i], ada_b[i])
            hc = modulate(ctx, norm_mix_g[i], c_sm, c_cm)
            lambda_init = 0.8 - 0.6 * math.exp(-0.3 * i)
            h = h + gm * even_mixer(hm, hc, w_in_even[j], q_norm_g[j], k_norm_g[j],
                                    lam_q1[j], lam_k1[j], lam_q2[j], lam_k2[j], subln_g[j],
                                    conv_w[j], w_out_even[j], cos, sin, lambda_init)
            hf = modulate(h, norm_ffn_g[i], sf, cf)
            h = h + gf * swiglu(hf, ffn_w_gate[j], ffn_w_up[j], ffn_w_down[j])
        else:
            h = h + gm * sgu_mixer(hm, sgu_w_in[j], sgu_ln_g[j], sgu_ln_b[j],
                                   sgu_w_s[j], sgu_b_s[j], sgu_w_out[j])
            hf = modulate(h, norm_ffn_g[i], sf, cf)
            h = h + gf * moe_swiglu(hf, router_w[j], moe_w_gate[j], moe_w_up[j], moe_w_down[j])
    return h
```

```python
import contextlib
import math
import numpy as np
import concourse.bass as bass
import concourse.mybir as mybir
from concourse.bass_utils import run_bass_kernel_spmd

F32 = mybir.dt.float32
BF16 = mybir.dt.bfloat16
AF = mybir.ActivationFunctionType
ALU = mybir.AluOpType
AX = mybir.AxisListType

D = 1024
S = 2048
LC = 256
NT = 4
NV = 416
LAMBDA_INIT0 = 0.8 - 0.6 * math.exp(0.0)


def I(name, *args, **kw):
    return (name, args, kw)


class Buf:
    __slots__ = ("name", "w", "r", "dsem", "dcount")

    def __init__(self, name):
        self.name = name
        self.w = None
        self.r = {}
        self.dsem = None
        self.dcount = 0


class Prog:
    ENGS = ("pe", "act", "dve", "pool", "sp")

    def __init__(self, nc, stack):
        self.nc = nc
        self.stack = stack
        self.sem = {}
        self.count = {}
        self.seen = {e: {} for e in self.ENGS}
        self.q = {e: [] for e in self.ENGS}
        self.pending = {e: False for e in self.ENGS}
        for e in self.ENGS:
            self.sem[e] = stack.enter_context(nc.semaphore("prog_" + e))
            self.count[e] = 0
        self.bufs = {}
        self.dma_bufs = {}
        self.dma_toks = {}
        self.dma_free = {}
        self.dma_all = []
        self.epoch = 0
        self.maxcount = 0

    def B(self, *key):
        b = self.bufs.get(key)
        if b is None:
            b = Buf("_".join(str(k) for k in key))
            self.bufs[key] = b
        return b

    def _dsem(self, b, eng):
        key = (eng, b.name)
        ent = self.dma_bufs.get(key)
        if ent is None:
            free = self.dma_free.setdefault(eng, [])
            if free:
                ent = free.pop()
            else:
                self.ndsem = getattr(self, "ndsem", 0) + 1
                ent = [self.stack.enter_context(self.nc.semaphore("dma_%s_%d" % (eng, self.ndsem))), 0]
                self.dma_all.append((eng, ent))
            self.dma_bufs[key] = ent
        return ent

    def _waits(self, eng, reads, writes):
        need = {}

        def add(tok):
            if tok is None:
                return
            k, v = tok
            if need.get(k, 0) < v:
                need[k] = v

        for b in reads:
            add(b.w)
        for b in writes:
            add(b.w)
            for k, v in b.r.items():
                add((k, v))
        out = []
        for k, v in need.items():
            if k == eng and eng == "pe":
                continue
            if self.seen[eng].get(k, 0) >= v:
                continue
            self.seen[eng][k] = v
            out.append((k, v))
        return out

    def _semof(self, k):
        return self.sem[k] if isinstance(k, str) else k

    def op(self, eng, fn, reads=(), writes=(), inc=True):
        for k, v in self._waits(eng, reads, writes):
            self.q[eng].append(("wait", self._semof(k), v))
        if inc:
            self.count[eng] += 1
            tok = (eng, self.count[eng])
            self.pending[eng] = False
        else:
            tok = (eng, self.count[eng] + 1)
            self.pending[eng] = True
        self.q[eng].append(("op", fn, inc, self.sem[eng]))
        for b in reads:
            if b.r.get(eng, 0) < tok[1]:
                b.r[eng] = tok[1]
        for b in writes:
            b.w = tok
            b.r = {}
        return tok

    def dma(self, eng, out_ap, in_ap, reads=(), writes=(), sembuf=None):
        sb = sembuf if sembuf is not None else (writes[0] if writes else reads[0])
        ent = self._dsem(sb, eng)
        for k, v in self._waits(eng, reads, writes):
            self.q[eng].append(("wait", self._semof(k), v))
        ent[1] += 16
        tok = (ent[0], ent[1])
        self.dma_toks[id(ent[0])] = tok
        self.q[eng].append(("dma", out_ap, in_ap, ent[0]))
        for b in reads:
            if b.r.get(tok[0], 0) < tok[1]:
                b.r[tok[0]] = tok[1]
        for b in writes:
            b.w = tok
            b.r = {}
        return tok

    def wait_tok(self, eng, tok):
        k, v = tok
        if self.seen[eng].get(k, 0) >= v:
            return
        self.seen[eng][k] = v
        self.q[eng].append(("wait", self._semof(k), v))

    def barrier(self):
        for e in self.ENGS:
            assert not self.pending[e], e
        for e in self.ENGS:
            for o in self.ENGS:
                if self.count[o] > 0:
                    self.wait_tok(e, (o, self.count[o]))
            for tok in self.dma_toks.values():
                self.wait_tok(e, tok)
        self.bufs = {}
        self.epoch += 1
        self.dma_bufs = {}
        self.dma_free = {}
        for eng, ent in self.dma_all:
            self.dma_free.setdefault(eng, []).append(ent)

    def fresh(self):
        self.nfresh = getattr(self, "nfresh", 0) + 1
        for e in self.ENGS:
            self.sem[e] = self.stack.enter_context(self.nc.semaphore("prog_%s_f%d" % (e, self.nfresh)))
            self.count[e] = 0
            self.seen[e] = {}
        self.dma_bufs = {}
        self.dma_free = {}
        self.dma_all = []
        self.dma_toks = {}
        self.bufs = {}

    def dyn_if(self, cond_ap, thr):
        for e in self.ENGS:
            self.q[e].append(("if", cond_ap, thr))

    def skip_begin(self, cond_ap, thr):
        self._skip = {"count": dict(self.count), "ndma": len(self.dma_all),
                      "bump": {e: [] for e in self.ENGS}}
        self.dma_free = {}
        self.dma_bufs = {}
        for e in self.ENGS:
            self.q[e].append(("if", cond_ap, thr))
            self.q[e].append(("bump", self._skip["bump"][e], self.sem[e], self.count[e]))
            self.q[e].append(("else",))

    def skip_end(self):
        sk = self._skip
        for e in self.ENGS:
            d = self.count[e] - sk["count"][e]
            if d:
                sk["bump"][e].append((self.sem[e], d))
        for eng, ent in self.dma_all[sk["ndma"]:]:
            self.dma_toks.pop(id(ent[0]), None)
        del self.dma_all[sk["ndma"]:]
        self.dma_bufs = {}
        for e in self.ENGS:
            self.q[e].append(("endif",))

    def dyn_else(self):
        for e in self.ENGS:
            self.q[e].append(("else",))

    def dyn_end(self):
        for e in self.ENGS:
            self.q[e].append(("endif",))

    def emit(self):
        nc = self.nc
        for e in self.ENGS:
            assert not self.pending[e], f"engine {e} has trailing un-inc'd op"
        engobj = {"pe": "tensor", "act": "scalar", "dve": "vector", "pool": "gpsimd", "sp": "sync"}
        with nc.Block() as block:
            for e in self.ENGS:
                items = self.q[e]
                sem = self.sem[e]

                def body(engine, items=items, sem=sem):
                    guards = []
                    nreg = [0]
                    for it in items:
                        if it[0] == "if":
                            nreg[0] += 1
                            rg = engine.register("dynr%d" % nreg[0])
                            r = rg.__enter__()
                            engine.load(r, it[1], bass_reorder=False)
                            g = engine.If_lt(r, it[2])
                            g.__enter__()
                            guards.append(g)
                            guards.append(rg)
                        elif it[0] == "bump":
                            if it[3] > 0:
                                engine.wait_ge(it[2], it[3])
                            for bsem, amt in it[1]:
                                while amt > 0:
                                    a = min(amt, 4096)
                                    engine.sem_inc(bsem, a)
                                    amt -= a
                        elif it[0] == "else":
                            rg = guards.pop()
                            guards.pop().__exit__(None, None, None)
                            g = engine.Else()
                            g.__enter__()
                            guards.append(g)
                            guards.append(rg)
                        elif it[0] == "endif":
                            rg = guards.pop()
                            guards.pop().__exit__(None, None, None)
                            rg.__exit__(None, None, None)
                        elif it[0] == "wait":
                            engine.wait_ge(it[1], it[2])
                        elif it[0] == "op":
                            name, args, kw = it[1]
                            ins = getattr(engine, name)(*args, **kw)
                            if it[2]:
                                ins.then_inc(it[3], 1)
                        else:
                            engine.dma_start(out=it[1], in_=it[2]).then_inc(it[3], 16)

                getattr(block, engobj[e])(body)


def build_program(upto=4):
    nc = bass.Bass("TRN2", target_bir_lowering=False)

    def din(name, shape):
        return nc.dram_tensor(name, list(shape), F32, kind="ExternalInput").ap()

    xT = din("xT", [D, S])
    ctxT = din("ctxT", [D, LC])
    vecs = din("vecs", [128, NV])
    lnb = din("lnb", [128, 2048])
    bsd = din("bs", [1, 512])
    rope = din("rope", [128, 2, S])
    mats = din("mats", [128, 3, 128])
    seld = din("sel", [8, 1024])
    cst2 = din("cst2", [128, 776])
    ada_w = din("ada_w", [2, D, 6 * D])
    w_in = din("w_in", [D, 3072])
    w_out = din("w_out", [D, D])
    ffg = din("ffn_g", [D, 2816])
    ffu = din("ffn_u", [D, 2816])
    ffd = din("ffn_d", [2816, D])
    sgi = din("sgu_in", [D, 2048])
    wsTd = din("wsT", [128, 512])
    sgo = din("sgu_out", [D, D])
    rtr = din("router", [D, 8])
    mg = din("moe_g", [8, 14, 128, 2048])
    mu = din("moe_u", [8, 14, 128, 2048])
    md = din("moe_d", [8, 14, 128, 2048])
    yT = nc.dram_tensor("yT", [D, S], F32, kind="ExternalOutput").ap()

    with contextlib.ExitStack() as st:
        P = Prog(nc, st)
        B = P.B
        AW = 207 * 256
        arena = st.enter_context(nc.sbuf_tensor("arena", [128, AW], F32))
        psum = st.enter_context(nc.psum_tensor("psum", [128, 8, 512], F32))

        def Fv(kib, *shape):
            n = int(np.prod(shape))
            w0 = int(round(kib * 256))
            v = arena[:, w0:w0 + n]
            if len(shape) == 2:
                return v.rearrange("p (a b) -> p a b", a=shape[0])
            if len(shape) == 3:
                return v.rearrange("p (a b c) -> p a b c", a=shape[0], b=shape[1])
            return v

        def Hv(kib, *shape):
            n = int(np.prod(shape))
            assert n % 2 == 0
            w0 = int(round(kib * 256))
            v = arena[:, w0:w0 + n // 2].bitcast(BF16)
            if len(shape) == 2:
                return v.rearrange("p (a b) -> p a b", a=shape[0])
            if len(shape) == 3:
                return v.rearrange("p (a b c) -> p a b c", a=shape[0], b=shape[1])
            return v

        def PS(b, w=512):
            return psum[:, b, 0:w]

        hT = Fv(0, 8, S)
        xn = Hv(64, 8, S)
        xc = Hv(96, 8, LC)
        vt = Fv(100, NV)
        pv = Fv(101.75, 128)
        ones = Hv(102.25, 128)
        bones = Hv(102.5, 128)
        prot = Hv(102.75, 128)
        ident = Fv(103, 128)
        mod = Fv(103.5, 2, 96)
        scb = Hv(104.25, 8, 2)
        lamt = Fv(104.5, 64)
        eps = vt[:, 159:160]

        def PVC(layer, which, kc):
            return pv[:, layer * 48 + which * 8 + kc: layer * 48 + which * 8 + kc + 1]
        NEG_LAM = pv[:, 112:113]
        GSUB = pv[:, 113:114]

        def mmg(out_ap, out_buf, terms, reads):
            n = len(terms)
            for i, (l, r) in enumerate(terms):
                P.op("pe", (I("matmul", out_ap, l, r, start=(i == 0), stop=(i == n - 1))),
                     reads=reads, writes=[out_buf], inc=(i == n - 1))

        def slab_load(dst, src_rows, bufkey):
            P.dma("pool", dst, src_rows.rearrange("(kc p) n -> p kc n", p=128), writes=[bufkey])

        MT = 190

        def modulate(src_fn, src_buf, W, ntiles, dst_fn, dst_buf, gs_fn, s_fn, f32_fn=None, f32_buf=None):
            sq = Hv(MT, 2, 512)
            rt = Fv(MT + 2, 512)
            tt = Fv(MT + 4, 2, 512)
            for n in range(ntiles):
                for kc in range(8):
                    sb = sq[:, kc % 2, 0:W]
                    P.op("act", (I("activation", sb, src_fn(kc, n), AF.Square)),
                         reads=[src_buf(kc, n)], writes=[B("msq", kc % 2)])
                    P.op("pe", (I("matmul", PS(7, W), ones, sb, start=(kc == 0), stop=(kc == 7))),
                         reads=[B("msq", kc % 2), B("ones")], writes=[B("ps", 7)])
                P.op("act", (I("activation", rt[:, 0:W], PS(7, W), AF.Sqrt, bias=eps, scale=1.0 / D)),
                     reads=[B("ps", 7), B("vt")], writes=[B("mrt")])
                P.op("dve", (I("reciprocal", rt[:, 0:W], rt[:, 0:W])), reads=[B("mrt")], writes=[B("mrt")])
                for kc in range(8):
                    tb = tt[:, kc % 2, 0:W]
                    P.op("dve", (I("scalar_tensor_tensor", tb, src_fn(kc, n), gs_fn(kc), rt[:, 0:W], ALU.mult, ALU.mult)),
                         reads=[src_buf(kc, n), B("mrt"), B("pv")], writes=[B("mt", kc % 2)])
                    if f32_fn is None:
                        P.op("act", (I("activation", dst_fn(kc, n), tb, AF.Identity, bias=s_fn(kc))),
                             reads=[B("mt", kc % 2), B("pv")], writes=[dst_buf(kc, n)])
                    else:
                        P.op("act", (I("activation", f32_fn(kc, n), tb, AF.Identity, bias=s_fn(kc))),
                             reads=[B("mt", kc % 2), B("pv")], writes=[f32_buf(kc, n)])
                        P.op("dve", (I("tensor_copy", dst_fn(kc, n), f32_fn(kc, n))),
                             reads=[f32_buf(kc, n)], writes=[dst_buf(kc, n)])

        def hsrc(kc, n):
            return hT[:, kc, n * 512:(n + 1) * 512]

        def hbuf(kc, n):
            return B("h", kc, n)

        def xdst(kc, n):
            return xn[:, kc, n * 512:(n + 1) * 512]

        def xbuf(kc, n):
            return B("xn", kc, n)

        P.dma("sp", vt, vecs, writes=[B("vt")])
        for kc in range(8):
            P.dma("sp", hT[:, kc, :], xT[kc * 128:(kc + 1) * 128, :], writes=[B("h", kc, n) for n in range(NT)], sembuf=B("hload", kc % 4))
        P.dma("sp", ident, mats[:, 2, :], writes=[B("ident")])
        P.dma("pool", bones, mats[:, 0, :], writes=[B("bones")])
        P.dma("pool", prot, mats[:, 1, :], writes=[B("prot")])
        P.op("pool", I("memset", ones, 1.0), writes=[B("ones")])
        P.op("act", I("activation", scb[:, :, 0], vt[:, 0:8], AF.Silu), reads=[B("vt")], writes=[B("scb")])
        P.op("act", I("activation", scb[:, :, 1], vt[:, 8:16], AF.Silu), reads=[B("vt")], writes=[B("scb")])
        ring0 = [Hv(106 + 8 * i, 8, 512) for i in range(4)]

        def ada_dma(layer, s, slot, sbuf):
            slab_load(slot, ada_w[layer, :, s * 512:(s + 1) * 512], sbuf)

        def ada_mm(layer, s, slot, sbuf, bank):
            for mm in range(4):
                m = s * 4 + mm
                mmg(psum[:, bank, m * 2:m * 2 + 2], B("ps", bank),
                    [(slot[:, kc, mm * 128:(mm + 1) * 128], scb[:, kc, :]) for kc in range(8)],
                    [sbuf, B("scb")])

        def ada_add(layer, bank):
            ab = vt[:, 16 + 48 * layer:64 + 48 * layer]
            for col in range(2):
                P.op("dve", (I("tensor_tensor",
                    mod[:, layer, :].rearrange("p (m c) -> p m c", c=2)[:, :, col],
                    psum[:, bank, 0:96].rearrange("p (m c) -> p m c", c=2)[:, :, col], ab, ALU.add)),
                    reads=[B("ps", bank), B("vt")], writes=[B("mod", layer)])

        for s in range(12):
            ada_dma(0, s, ring0[s % 4], B("r0", s % 4))
            ada_mm(0, s, ring0[s % 4], B("r0", s % 4), 0)
        ada_add(0, 0)

        def modv(layer, chunk, col=0):
            return mod[:, layer, :].rearrange("p (m c) -> p m c", c=2)[:, chunk * 8:(chunk + 1) * 8, col]

        def ada_derive(layer):
            nmg = vt[:, 112 + 8 * layer:120 + 8 * layer]
            nfg = vt[:, 128 + 8 * layer:136 + 8 * layer]
            b0 = layer * 48
            ops = [
                (I("scalar_tensor_tensor", pv[:, b0:b0 + 8], modv(layer, 1), 1.0, nmg, ALU.add, ALU.mult)),
                (I("tensor_copy", pv[:, b0 + 8:b0 + 16], modv(layer, 0))),
                (I("tensor_copy", pv[:, b0 + 16:b0 + 24], modv(layer, 2))),
                (I("scalar_tensor_tensor", pv[:, b0 + 24:b0 + 32], modv(layer, 4), 1.0, nfg, ALU.add, ALU.mult)),
                (I("tensor_copy", pv[:, b0 + 32:b0 + 40], modv(layer, 3))),
                (I("tensor_copy", pv[:, b0 + 40:b0 + 48], modv(layer, 5))),
            ]
            for f in ops:
                P.op("dve", f, reads=[B("mod", layer), B("vt")], writes=[B("pv")])

        ada_derive(0)
        P.op("dve", I("scalar_tensor_tensor", pv[:, 96:104], modv(0, 1, 1), 1.0, vt[:, 112:120], ALU.add, ALU.mult),
             reads=[B("mod", 0), B("vt")], writes=[B("pv")])
        P.op("dve", I("tensor_copy", pv[:, 104:112], modv(0, 0, 1)), reads=[B("mod", 0)], writes=[B("pv")])
        P.op("dve", I("tensor_tensor", lamt[:, 0:64], vt[:, 160:224], vt[:, 224:288], ALU.mult), reads=[B("vt")], writes=[B("lamt")])
        P.op("dve", I("reduce_sum", pv[:, 114:115], lamt[:, 0:64], AX.X), reads=[B("lamt")], writes=[B("pv")])
        P.op("dve", I("tensor_tensor", lamt[:, 0:64], vt[:, 288:352], vt[:, 352:416], ALU.mult), reads=[B("vt"), B("pv")], writes=[B("lamt")])
        P.op("dve", I("reduce_sum", pv[:, 115:116], lamt[:, 0:64], AX.X), reads=[B("lamt")], writes=[B("pv")])
        P.op("act", I("activation", pv[:, 116:118], pv[:, 114:116], AF.Exp), reads=[B("pv")], writes=[B("pv2")])
        P.op("dve", I("tensor_tensor", pv[:, 118:119], pv[:, 117:118], pv[:, 116:117], ALU.subtract), reads=[B("pv2")], writes=[B("pv3")])
        P.op("dve", I("tensor_scalar", NEG_LAM, pv[:, 118:119], -LAMBDA_INIT0, None, ALU.add), reads=[B("pv3")], writes=[B("pv")])
        P.op("dve", I("tensor_scalar", GSUB, vt[:, 146:147], 1.0 - LAMBDA_INIT0, None, ALU.mult), reads=[B("vt")], writes=[B("pv")])
        P.barrier()

        if upto >= 1:
            modulate(hsrc, hbuf, 512, NT, xdst, xbuf, lambda kc: PVC(0, 0, kc), lambda kc: PVC(0, 1, kc))
            c32 = Fv(106, 8, LC)
            for kc in range(8):
                P.dma("sp", c32[:, kc, :], ctxT[kc * 128:(kc + 1) * 128, :], writes=[B("c32", kc)], sembuf=B("c32ld"))
            for kc in range(8):
                B("c32", kc).w = B("c32", 7).w
            modulate(lambda kc, n: c32[:, kc, :], lambda kc, n: B("c32", kc), LC, 1,
                     lambda kc, n: xc[:, kc, :], lambda kc, n: B("xc", kc),
                     lambda kc: pv[:, 96 + kc:97 + kc], lambda kc: pv[:, 104 + kc:105 + kc])
            P.barrier()

            qT = Hv(106, 4, S)
            kT = Hv(122, 4, S + LC)
            vtok = Hv(140, 18, 512)
            wq = Hv(158, 8, 512)
            wk = Hv(166, 8, 512)
            wv = Hv(174, 8, 512)
            cs = Fv(182, 2, 2, 512)
            qg = Hv(190, 2, 512)
            sqq = Hv(192, 2, 512)
            rtq = Fv(194, 512)
            t1 = Fv(196, 512)
            t2 = Fv(198, 512)
            slab_load(wk, w_in[:, 512:1024], B("wk"))
            slab_load(wv, w_in[:, 1024:1536], B("wv"))
            slab_load(wq, w_in[:, 0:512], B("wq"))
            ectr = [0]

            def qk_epilogue(bank, W, gcol, dst, dst_buf, n, use_rope):
                i = ectr[0] % 2
                ectr[0] += 1
                A = PS(bank, W)
                P.op("act", (I("activation", qg[:, i, 0:W], A, AF.Identity, scale=vt[:, gcol:gcol + 1])),
                     reads=[B("ps", bank), B("vt")], writes=[B("qg", i)])
                P.op("act", (I("activation", sqq[:, i, 0:W], A, AF.Square)),
                     reads=[B("ps", bank)], writes=[B("sqq", i)])
                bs_, bc_ = 2 + i, 4 + i
                mmg(PS(bs_, W), B("ps", bs_), [(bones, sqq[:, i, 0:W])], [B("bones"), B("sqq", i)])
                if use_rope:
                    mmg(PS(bc_, W), B("ps", bc_), [(prot, qg[:, i, 0:W])], [B("prot"), B("qg", i)])
                P.op("act", (I("activation", rtq[:, 0:W], PS(bs_, W), AF.Sqrt, bias=eps, scale=1.0 / 64)),
                     reads=[B("ps", bs_), B("vt")], writes=[B("rtq")])
                P.op("dve", (I("reciprocal", rtq[:, 0:W], rtq[:, 0:W])), reads=[B("rtq")], writes=[B("rtq")])
                if use_rope:
                    cb = n % 2
                    P.op("dve", (I("tensor_tensor", t1[:, 0:W], qg[:, i, 0:W], cs[:, cb, 0, :], ALU.mult)),
                         reads=[B("qg", i), B("cs", cb)], writes=[B("t1")])
                    P.op("dve", (I("tensor_tensor", t2[:, 0:W], PS(bc_, W), cs[:, cb, 1, :], ALU.mult)),
                         reads=[B("ps", bc_), B("cs", cb)], writes=[B("t2")])
                    P.op("dve", (I("tensor_tensor", t1[:, 0:W], t1[:, 0:W], t2[:, 0:W], ALU.add)),
                         reads=[B("t1"), B("t2")], writes=[B("t1")])
                    P.op("dve", (I("tensor_tensor", dst, t1[:, 0:W], rtq[:, 0:W], ALU.mult)),
                         reads=[B("t1"), B("rtq")], writes=[dst_buf])
                else:
                    P.op("dve", (I("tensor_tensor", dst, qg[:, i, 0:W], rtq[:, 0:W], ALU.mult)),
                         reads=[B("qg", i), B("rtq")], writes=[dst_buf])

            actr = [0]
            for hd in range(4):
                bank = actr[0] % 2
                actr[0] += 1
                mmg(PS(bank, LC), B("ps", bank), [(wk[:, kc, hd * 128:(hd + 1) * 128], xc[:, kc, :]) for kc in range(8)],
                    [B("wk")] + [B("xc", kc) for kc in range(8)])
                qk_epilogue(bank, LC, 145, kT[:, hd, 0:LC], B("kT", hd, "c"), 0, False)
            vctr = [0]

            def v_chunk(src_fn, src_bufs, j):
                bank = 6 + vctr[0] % 2
                vctr[0] += 1
                mmg(PS(bank), B("ps", bank), [(src_fn(kc), wv[:, kc, :]) for kc in range(8)], [B("wv")] + src_bufs)
                P.op("act", (I("activation", vtok[:, j, :], PS(bank), AF.Copy)), reads=[B("ps", bank)], writes=[B("vtok", j)])

            for j in range(2):
                v_chunk(lambda kc, j=j: xc[:, kc, j * 128:(j + 1) * 128], [B("xc", kc) for kc in range(8)], j)
            for n in range(NT):
                cb = n % 2
                P.dma("sp", cs[:, cb, :, :], rope[:, :, n * 512:(n + 1) * 512], writes=[B("cs", cb)])
                for which in range(2):
                    w_ = wk if which == 0 else wq
                    wb = B("wk") if which == 0 else B("wq")
                    for hd in range(4):
                        bank = actr[0] % 2
                        actr[0] += 1
                        mmg(PS(bank), B("ps", bank), [(w_[:, kc, hd * 128:(hd + 1) * 128], xdst(kc, n)) for kc in range(8)],
                            [wb] + [xbuf(kc, n) for kc in range(8)])
                        if which == 0:
                            qk_epilogue(bank, 512, 145, kT[:, hd, LC + n * 512:LC + (n + 1) * 512], B("kT", hd, n), n, True)
                        else:
                            qk_epilogue(bank, 512, 144, qT[:, hd, n * 512:(n + 1) * 512], B("qT", hd, n), n, True)
                for jj in range(4):
                    j = n * 4 + jj
                    v_chunk(lambda kc, j=j: xn[:, kc, j * 128:(j + 1) * 128], [xbuf(kc, n) for kc in range(8)], 2 + j)
            P.barrier()

            yA = Hv(158, 4, S)
            pT = Hv(174, 4, 512)
            r0 = Fv(178, 512)
            r1 = Fv(180, 512)
            aa = Fv(182, 512)
            bb = Fv(184, 512)
            sqa = Hv(186, 512)
            NJ = 18
            for n in range(NT):
                for hd in range(4):
                    steps = [(c, j) for c in range(2) for j in range(NJ)]

                    def qk(i):
                        c, j = steps[i]
                        sb_ = i % 3
                        mmg(PS(sb_), B("ps", sb_),
                            [(kT[c * 64:(c + 1) * 64, hd, j * 128:(j + 1) * 128], qT[c * 64:(c + 1) * 64, hd, n * 512:(n + 1) * 512])], [])
                        P.op("act", (I("activation", pT[:, i % 4, :], PS(sb_), AF.Exp, scale=0.125)),
                             reads=[B("ps", sb_)], writes=[B("pT", i % 4)])

                    def pvz(i):
                        c, j = steps[i]
                        ob, zb = 3 + c, 5 + c
                        P.op("pe", (I("matmul", PS(ob), vtok[:, j, hd * 128:(hd + 1) * 128], pT[:, i % 4, :], start=(j == 0), stop=(j == NJ - 1))),
                             reads=[B("pT", i % 4)], writes=[B("ps", ob)], inc=False)
                        P.op("pe", (I("matmul", PS(zb), ones, pT[:, i % 4, :], start=(j == 0), stop=(j == NJ - 1))),
                             reads=[B("pT", i % 4)], writes=[B("ps", zb)], inc=True)

                    qk(0)
                    qk(1)
                    for i in range(len(steps)):
                        pvz(i)
                        if i + 2 < len(steps):
                            qk(i + 2)
                    P.op("dve", I("reciprocal", r0, PS(5)), reads=[B("ps", 5)], writes=[B("r0")])
                    P.op("dve", I("reciprocal", r1, PS(6)), reads=[B("ps", 6)], writes=[B("r1")])
                    P.op("dve", I("tensor_tensor", aa, PS(3), r0, ALU.mult), reads=[B("ps", 3), B("r0")], writes=[B("aa")])
                    P.op("dve", I("tensor_tensor", bb, PS(4), r1, ALU.mult), reads=[B("ps", 4), B("r1")], writes=[B("bb")])
                    P.op("dve", I("scalar_tensor_tensor", aa, bb, NEG_LAM, aa, ALU.mult, ALU.add), reads=[B("aa"), B("bb")], writes=[B("aa")])
                    P.op("act", I("activation", sqa, aa, AF.Square), reads=[B("aa")], writes=[B("sqa")])
                    mmg(PS(7), B("ps", 7), [(ones, sqa)], [B("sqa")])
                    P.op("act", I("activation", r0, PS(7), AF.Sqrt, bias=eps, scale=1.0 / 128), reads=[B("ps", 7)], writes=[B("r0")])
                    P.op("dve", I("reciprocal", r0, r0), reads=[B("r0")], writes=[B("r0")])
                    P.op("dve", (I("scalar_tensor_tensor", yA[:, hd, n * 512:(n + 1) * 512], aa, GSUB, r0, ALU.mult, ALU.mult)),
                         reads=[B("aa"), B("r0")], writes=[B("yA", hd, n)])
            P.barrier()

            yC = Hv(106, 4, S)
            cslab = [Hv(122 + 6 * i, 8, 384) for i in range(2)]
            g3 = Hv(134, 3, S)
            zz = Fv(146, 2050)
            acc = Fv(174, S)
            P.op("pool", I("memset", zz[:, 0:1], 0.0), writes=[B("zz")])
            P.op("pool", I("memset", zz[:, 2049:2050], 0.0), writes=[B("zz")])
            cctr = 0
            for c in range(4):
                sl = cslab[c % 2]
                for t3 in range(3):
                    col = 1536 + 512 * t3 + c * 128
                    P.dma("pool", sl[:, :, t3 * 128:(t3 + 1) * 128], w_in[:, col:col + 128].rearrange("(kc p) n -> p kc n", p=128),
                          writes=[B("cslab", c % 2)])
                for t3 in range(3):
                    for n in range(NT):
                        bank = cctr % 4
                        cctr += 1
                        mmg(PS(bank), B("ps", bank), [(sl[:, kc, t3 * 128:(t3 + 1) * 128], xdst(kc, n)) for kc in range(8)], [B("cslab", c % 2)])
                        P.op("act", (I("activation", g3[:, t3, n * 512:(n + 1) * 512], PS(bank), AF.Copy)),
                             reads=[B("ps", bank)], writes=[B("g3", t3)])
                w0 = vt[:, 147 + c:148 + c]
                w1 = vt[:, 151 + c:152 + c]
                w2 = vt[:, 155 + c:156 + c]
                P.op("dve", I("tensor_tensor", zz[:, 1:2049], g3[:, 1, :], g3[:, 2, :], ALU.mult), reads=[B("g3", 1), B("g3", 2)], writes=[B("zz")])
                P.op("dve", (I("tensor_scalar", acc, zz[:, 1:2049], w1, None, ALU.mult)), reads=[B("zz")], writes=[B("acc")])
                P.op("dve", (I("scalar_tensor_tensor", acc, zz[:, 0:2048], w0, acc, ALU.mult, ALU.add)), reads=[B("zz"), B("acc")], writes=[B("acc")])
                P.op("dve", (I("scalar_tensor_tensor", acc, zz[:, 2:2050], w2, acc, ALU.mult, ALU.add)), reads=[B("zz"), B("acc")], writes=[B("acc")])
                P.op("dve", (I("tensor_tensor", yC[:, c, :], acc, g3[:, 0, :], ALU.mult)), reads=[B("acc"), B("g3", 0)], writes=[B("yC", c)])
            P.barrier()

            wo = [Hv(122 + 8 * i, 8, 512) for i in range(2)]
            for s in range(2):
                slab_load(wo[s], w_out[:, s * 512:(s + 1) * 512], B("wo", s))
            octr = 0
            for m in range(8):
                for n in range(NT):
                    bank = octr % 4
                    octr += 1
                    terms = []
                    for kc in range(8):
                        ysrc = yA[:, kc, n * 512:(n + 1) * 512] if kc < 4 else yC[:, kc - 4, n * 512:(n + 1) * 512]
                        terms.append((wo[m // 4][:, kc, (m % 4) * 128:(m % 4 + 1) * 128], ysrc))
                    mmg(PS(bank), B("ps", bank), terms, [B("wo", m // 4)])
                    P.op("dve", (I("scalar_tensor_tensor", hsrc(m, n), PS(bank), PVC(0, 2, m), hsrc(m, n), ALU.mult, ALU.add)),
                         reads=[B("ps", bank), hbuf(m, n)], writes=[hbuf(m, n)])
            P.barrier()

        def ffn(layer, groups, gate_src, up_src, down_src, gate_fn=None, hook=None, dbanks=4, ring_base=138, actb_base=198):
            ring = [Hv(ring_base + 8 * i, 8, 512) for i in range(6)]
            actb = Hv(actb_base, 2, 4, 512)
            sS = Fv(106, 2, 512)
            sT = Fv(110, 2, 512)
            rc = [0]
            ac = [0]
            fc = [0]
            dc = [0]

            def load_group(gi):
                f0, nf = groups[gi]
                slots = []
                for t in range(3):
                    si_ = rc[0] % 6
                    rc[0] += 1
                    if t < 2:
                        src = (gate_src if t == 0 else up_src)(gi)
                        P.dma("pool", ring[si_][:, :, 0:nf * 128], src.rearrange("(kc p) n -> p kc n", p=128), writes=[B("ring", si_)])
                    else:
                        dv = ring[si_].rearrange("p a b -> p (a b)")[:, 0:nf * 1024].rearrange("p (f n) -> p f n", n=1024)
                        P.dma("pool", dv, down_src(gi).rearrange("(f p) n -> p f n", p=128), writes=[B("ring", si_)])
                    slots.append(si_)
                return slots

            def emit_down(wd_, sd, nf, ab, n):
                for m in range(8):
                    db = 4 + dc[0] % dbanks
                    dc[0] += 1
                    mmg(PS(db), B("ps", db), [(wd_[:, f, m * 128:(m + 1) * 128], actb[:, ab, f, :]) for f in range(nf)],
                        [B("ring", sd)] + [B("act", ab, f) for f in range(nf)])
                    P.op("dve", (I("scalar_tensor_tensor", hsrc(m, n), PS(db), PVC(layer, 5, m), hsrc(m, n), ALU.mult, ALU.add)),
                         reads=[B("ps", db), hbuf(m, n)], writes=[hbuf(m, n)])

            pend = load_group(0)
            pdown = None
            for gi in range(len(groups)):
                f0, nf = groups[gi]
                sg, su, sd = pend
                wg_, wu_ = ring[sg], ring[su]
                wd_ = ring[sd].rearrange("p a b -> p (a b)")[:, 0:nf * 1024].rearrange("p (f n) -> p f n", n=1024)
                for n in range(NT):
                    ab = ac[0] % 2
                    ac[0] += 1
                    if gate_fn is not None:
                        gap, gbuf = gate_fn(gi, n)
                    for f in range(nf):
                        k = fc[0] % 2
                        fc[0] += 1
                        gb_, ub_ = k, 2 + k
                        mmg(PS(gb_), B("ps", gb_), [(wg_[:, kc, f * 128:(f + 1) * 128], xdst(kc, n)) for kc in range(8)],
                            [B("ring", sg)] + [xbuf(kc, n) for kc in range(8)])
                        mmg(PS(ub_), B("ps", ub_), [(wu_[:, kc, f * 128:(f + 1) * 128], xdst(kc, n)) for kc in range(8)],
                            [B("ring", su)] + [xbuf(kc, n) for kc in range(8)])
                        P.op("act", (I("activation", sS[:, k, :], PS(gb_), AF.Silu)),
                             reads=[B("ps", gb_)], writes=[B("sS", k)])
                        if gate_fn is None:
                            P.op("dve", (I("tensor_tensor", actb[:, ab, f, :], sS[:, k, :], PS(ub_), ALU.mult)),
                                 reads=[B("sS", k), B("ps", ub_)], writes=[B("act", ab, f)])
                        else:
                            P.op("dve", (I("tensor_tensor", sT[:, k, :], sS[:, k, :], PS(ub_), ALU.mult)),
                                 reads=[B("sS", k), B("ps", ub_)], writes=[B("sT", k)])
                            P.op("pool", (I("tensor_tensor", actb[:, ab, f, :], sT[:, k, :], gap, ALU.mult)),
                                 reads=[B("sT", k), gbuf], writes=[B("act", ab, f)])
                    if pdown is not None:
                        emit_down(*pdown)
                    pdown = (wd_, sd, nf, ab, n)
                    if n == 0 and gi + 1 < len(groups):
                        pend = load_group(gi + 1)
                    if hook is not None:
                        hook(gi * NT + n)
            emit_down(*pdown)

        if upto >= 2:
            modulate(hsrc, hbuf, 512, NT, xdst, xbuf, lambda kc: PVC(0, 3, kc), lambda kc: PVC(0, 4, kc))
            groups = [(0, 4), (4, 4), (8, 4), (12, 4), (16, 4), (20, 2)]
            a1 = [Hv(114 + 8 * i, 8, 512) for i in range(3)]

            def ada1_hook(it):
                if it < 12:
                    ada_dma(1, it, a1[it % 3], B("a1", it % 3))
                if 2 <= it < 14:
                    ada_mm(1, it - 2, a1[(it - 2) % 3], B("a1", (it - 2) % 3), 7)

            ffn(0, groups,
                lambda gi: ffg[:, groups[gi][0] * 128:(groups[gi][0] + groups[gi][1]) * 128],
                lambda gi: ffu[:, groups[gi][0] * 128:(groups[gi][0] + groups[gi][1]) * 128],
                lambda gi: ffd[groups[gi][0] * 128:(groups[gi][0] + groups[gi][1]) * 128, :],
                hook=ada1_hook, dbanks=3)
            ada_add(1, 7)
            ada_derive(1)
            P.barrier()

        if upto >= 3:
            modulate(hsrc, hbuf, 512, NT, xdst, xbuf, lambda kc: PVC(1, 0, kc), lambda kc: PVC(1, 1, kc))
            uT = Hv(106, 8, S)
            ring = [Hv(138 + 8 * i, 8, 512) for i in range(4)]
            vtf = Fv(170, 1024)
            vsq = Fv(174, 1024)
            vtk = Hv(178, 2, 1024)
            gtb = Fv(182, 2, 1024)
            wsT = Hv(198, 512)
            bsr = Fv(199, 512)
            st_ = Fv(201, 8)
            stmp = Fv(202, 2, 128)
            P.dma("sp", gtb.rearrange("p a b -> p (a b)"), lnb, writes=[B("gtb")])
            P.dma("pool", wsT, wsTd, writes=[B("wsT")])
            P.dma("sp", bsr, bsd[0:1, :].partition_broadcast(128), writes=[B("bsr")])
            for s in range(4):
                slab_load(ring[s], sgi[:, s * 512:(s + 1) * 512], B("sring", s))
            sc_ = 0
            for m in range(8):
                for n in range(NT):
                    bank = sc_ % 2
                    sc_ += 1
                    mmg(PS(bank), B("ps", bank), [(ring[m // 4][:, kc, (m % 4) * 128:(m % 4 + 1) * 128], xdst(kc, n)) for kc in range(8)],
                        [B("sring", m // 4)] + [xbuf(kc, n) for kc in range(8)])
                    P.op("act", (I("activation", uT[:, m, n * 512:(n + 1) * 512], PS(bank), AF.Gelu_apprx_tanh)),
                         reads=[B("ps", bank)], writes=[B("uT", m, n)])
            import os as _os
            _vmode = int(_os.environ.get("SGU_V", "9"))
            for j in range(16 if _vmode > 0 else 0):
                n = j // 4
                for hf in range(2):
                    bank = 2 + hf
                    mmg(PS(bank), B("ps", bank), [(xn[:, kc, j * 128:(j + 1) * 128], ring[2 + hf][:, kc, :]) for kc in range(8)],
                        [B("sring", 2 + hf)] + [xbuf(kc, n) for kc in range(8)])
                    P.op("act", (I("activation", vtf[:, hf * 512:(hf + 1) * 512], PS(bank), AF.Gelu_apprx_tanh)),
                         reads=[B("ps", bank)], writes=[B("vtf", hf)])
                if _vmode < 2:
                    continue
                P.op("dve", I("reduce_sum", st_[:, 0:1], vtf, AX.X), reads=[B("vtf", 0), B("vtf", 1)], writes=[B("st0")])
                P.op("dve", I("tensor_scalar", st_[:, 1:2], st_[:, 0:1], -1.0 / 1024, None, ALU.mult), reads=[B("st0")], writes=[B("st1")])
                P.op("act", I("activation", vsq, vtf, AF.Square, bias=st_[:, 1:2]), reads=[B("vtf", 0), B("vtf", 1), B("st1")], writes=[B("vsq")])
                P.op("dve", I("reduce_sum", st_[:, 2:3], vsq, AX.X), reads=[B("vsq")], writes=[B("st2")])
                P.op("act", I("activation", st_[:, 3:4], st_[:, 2:3], AF.Sqrt, bias=eps, scale=1.0 / 1024), reads=[B("st2")], writes=[B("st3")])
                P.op("dve", I("reciprocal", st_[:, 4:5], st_[:, 3:4]), reads=[B("st3")], writes=[B("st4")])
                P.op("dve", I("tensor_tensor", st_[:, 5:6], st_[:, 1:2], st_[:, 4:5], ALU.mult), reads=[B("st4"), B("st1")], writes=[B("st5")])
                P.op("act", I("activation", vsq, vtf, AF.Identity, bias=st_[:, 5:6], scale=st_[:, 4:5]),
                     reads=[B("vtf", 0), B("vtf", 1), B("st5"), B("st4")], writes=[B("vsq")])
                P.op("dve", I("tensor_tensor", vsq, vsq, gtb[:, 0, :], ALU.mult), reads=[B("vsq"), B("gtb")], writes=[B("vsq")])
                vb = j % 2
                P.op("dve", (I("tensor_tensor", vtk[:, vb, :], vsq, gtb[:, 1, :], ALU.add)), reads=[B("vsq"), B("gtb")], writes=[B("vtk", vb)])
                if _vmode < 3:
                    continue
                for cc in range(8):
                    g = cc // 2
                    bank = 4 + (cc % 4)
                    oap = psum[:, bank, 0:128]
                    mmg(oap, B("ps", bank),
                        [(vtk[:, vb, cc * 128:(cc + 1) * 128], wsT[:, g * 128:(g + 1) * 128])],
                        [B("vtk", vb), B("wsT")])
                    P.op("dve", (I("tensor_tensor", stmp[:, cc % 2, :], oap, bsr[:, g * 128:(g + 1) * 128], ALU.add)),
                         reads=[B("ps", bank), B("bsr")], writes=[B("stmp", cc % 2)])
                    P.op("dve", (I("tensor_tensor", uT[:, cc, j * 128:(j + 1) * 128], uT[:, cc, j * 128:(j + 1) * 128], stmp[:, cc % 2, :], ALU.mult)),
                         reads=[B("stmp", cc % 2), B("uT", cc, n)], writes=[B("uT", cc, n)])
            for s in range(2):
                slab_load(ring[s], sgo[:, s * 512:(s + 1) * 512], B("sring", s))
            octr = 0
            for m in range(8):
                for n in range(NT):
                    bank = 6 + octr % 2
                    octr += 1
                    mmg(PS(bank), B("ps", bank), [(ring[m // 4][:, kc, (m % 4) * 128:(m % 4 + 1) * 128], uT[:, kc, n * 512:(n + 1) * 512]) for kc in range(8)],
                        [B("sring", m // 4)] + [B("uT", kc, n) for kc in range(8)])
                    P.op("dve", (I("scalar_tensor_tensor", hsrc(m, n), PS(bank), PVC(1, 2, m), hsrc(m, n), ALU.mult, ALU.add)),
                         reads=[B("ps", bank), hbuf(m, n)], writes=[hbuf(m, n)])
            P.barrier()

        if upto >= 4:
            CAP = 640
            NB = 5
            hf32 = Fv(106, 8, 512)
            rw = Fv(122, 8, 8)
            lg = Fv(96, 16, 8)
            m1 = Fv(96.5, 16)
            mk1 = Fv(96.75, 16, 8)
            l2 = Fv(97.25, 16, 8)
            m2 = Fv(97.75, 16)
            mk2 = Fv(98, 16, 8)
            g1 = Fv(98.5, 16)
            g2 = Fv(98.75, 16)
            dn = Fv(99, 16, 8)
            gT = Fv(138, 2048)
            sel = Fv(146, 1024)
            msk = Fv(151, 16, 8)
            tot = Fv(151.5, 16, 8)
            off = Fv(152, 16, 8)
            pos = Fv(152.5, 16, 8)
            posm = Fv(153, 16, 8)
            cntv = Fv(153.5, 8)
            maxc = Fv(153.625, 8)
            condi = arena[:, int(153.75 * 256):int(153.75 * 256) + 1].bitcast(mybir.dt.int32)
            tri = Fv(154, 128)
            ones32 = Fv(154.5, 128)
            iota = Fv(155, CAP)
            slotcol = Fv(157.5, 8)
            identb = Hv(157.625, 128)
            P.dma("sp", rw, rtr.rearrange("(kc p) e -> p kc e", p=128), writes=[B("rw")])
            P.dma("sp", sel[0:8, :], seld, writes=[B("sel")])
            P.dma("sp", tri, cst2[:, 0:128], writes=[B("tri")])
            P.dma("sp", iota, cst2[:, 128:128 + CAP], writes=[B("iota")])
            P.dma("sp", slotcol, cst2[:, 768:776], writes=[B("slotcol")])
            P.op("pool", I("memset", ones32, 1.0), writes=[B("ones32")])
            P.op("pool", I("tensor_copy", identb, ident), reads=[B("ident")], writes=[B("identb")])
            for n in range(NT):
                modulate(lambda kc, nn, n=n: hsrc(kc, n), lambda kc, nn, n=n: hbuf(kc, n), 512, 1,
                         lambda kc, nn, n=n: xdst(kc, n), lambda kc, nn, n=n: xbuf(kc, n),
                         lambda kc: PVC(1, 3, kc), lambda kc: PVC(1, 4, kc),
                         f32_fn=lambda kc, nn: hf32[:, kc, :], f32_buf=lambda kc, nn: B("hf32", kc))
                for jj in range(4):
                    j = n * 4 + jj
                    mmg(psum[:, 0, j * 8:(j + 1) * 8], B("ps", 0),
                        [(hf32[:, kc, jj * 128:(jj + 1) * 128], rw[:, kc, :]) for kc in range(8)],
                        [B("rw")] + [B("hf32", kc) for kc in range(8)])
            fl = lambda t: t.rearrange("p a b -> p (a b)")
            P.op("dve", I("tensor_copy", fl(lg), psum[:, 0, 0:128]), reads=[B("ps", 0)], writes=[B("lg")])
            P.op("dve", I("tensor_reduce", m1, lg, AX.X, ALU.max), reads=[B("lg")], writes=[B("m1")])
            P.op("dve", I("tensor_tensor", mk1, lg, m1.unsqueeze(2).to_broadcast([128, 16, 8]), ALU.is_equal), reads=[B("lg"), B("m1")], writes=[B("mk1")])
            P.op("dve", I("scalar_tensor_tensor", l2, mk1, -1.0e30, lg, ALU.mult, ALU.add), reads=[B("mk1"), B("lg")], writes=[B("l2")])
            P.op("dve", I("tensor_reduce", m2, l2, AX.X, ALU.max), reads=[B("l2")], writes=[B("m2")])
            P.op("dve", I("tensor_tensor", mk2, l2, m2.unsqueeze(2).to_broadcast([128, 16, 8]), ALU.is_equal), reads=[B("l2"), B("m2")], writes=[B("mk2")])
            P.op("dve", I("tensor_tensor", msk, mk1, mk2, ALU.add), reads=[B("mk1"), B("mk2")], writes=[B("msk")])
            P.op("dve", I("tensor_tensor", g2, m1, m2, ALU.subtract), reads=[B("m1"), B("m2")], writes=[B("g2")])
            P.op("act", I("activation", g1, g2, AF.Sigmoid), reads=[B("g2")], writes=[B("g1")])
            P.op("dve", I("tensor_scalar", g2, g1, -1.0, 1.0, ALU.mult, ALU.add), reads=[B("g1")], writes=[B("g2")])
            P.op("dve", I("tensor_tensor", mk1, mk1, g1.unsqueeze(2).to_broadcast([128, 16, 8]), ALU.mult), reads=[B("mk1"), B("g1"), B("msk")], writes=[B("mk1")])
            P.op("dve", I("tensor_tensor", mk2, mk2, g2.unsqueeze(2).to_broadcast([128, 16, 8]), ALU.mult), reads=[B("mk2"), B("g2"), B("msk")], writes=[B("mk2")])
            P.op("dve", I("tensor_tensor", dn, mk1, mk2, ALU.add), reads=[B("mk1"), B("mk2")], writes=[B("dn")])
            for j in range(16):
                bank = 1 + (j // 4) % 2
                mmg(psum[0:8, bank, (j % 4) * 128:(j % 4 + 1) * 128], B("ps", bank), [(dn[:, j, :], ident)], [B("dn"), B("ident")])
                if j % 4 == 3:
                    q4 = j // 4
                    P.op("act", (I("activation", gT[0:8, q4 * 512:(q4 + 1) * 512], psum[0:8, bank, :], AF.Copy)),
                         reads=[B("ps", bank)], writes=[B("gT")])
            mmg(psum[:, 3, 0:128], B("ps", 3), [(tri, fl(msk))], [B("tri"), B("msk")])
            mmg(psum[:, 4, 0:128], B("ps", 4), [(ones32, fl(msk))], [B("ones32"), B("msk")])
            P.op("dve", I("tensor_copy", fl(tot), psum[:, 4, 0:128]), reads=[B("ps", 4)], writes=[B("tot")])
            P.op("dve", I("memset", off[:, 0, :], 0.0), writes=[B("off")])
            for j in range(1, 16):
                P.op("dve", I("tensor_tensor", off[:, j, :], off[:, j - 1, :], tot[:, j - 1, :], ALU.add), reads=[B("off"), B("tot")], writes=[B("off")])
            P.op("dve", I("tensor_tensor", fl(pos), psum[:, 3, 0:128], fl(off), ALU.add), reads=[B("ps", 3), B("off")], writes=[B("pos")])
            P.op("dve", I("scalar_tensor_tensor", fl(posm), fl(pos), 1.0, fl(msk), ALU.add, ALU.mult), reads=[B("pos"), B("msk")], writes=[B("posm")])
            P.op("dve", I("tensor_scalar", fl(posm), fl(posm), -1.0, None, ALU.add), reads=[B("posm")], writes=[B("posm")])
            P.op("dve", I("tensor_tensor", cntv, off[:, 15, :], tot[:, 15, :], ALU.add), reads=[B("off"), B("tot")], writes=[B("cntv")])
            P.op("dve", I("tensor_reduce", maxc[:, 0:1], cntv, AX.X, ALU.max), reads=[B("cntv")], writes=[B("maxc")])
            P.op("dve", I("tensor_copy", condi, maxc[:, 0:1]), reads=[B("maxc")], writes=[B("condi")])
            import os as _o3
            NPASS = int(_o3.environ.get("NPASS", "4"))
            posp = Fv(151, NPASS, 128)
            cnti = arena[:, int(153.875 * 256):int(153.875 * 256) + 8].bitcast(mybir.dt.int32)
            P.op("dve", I("tensor_copy", cnti, cntv), reads=[B("cntv")], writes=[B("cnti")])
            for p_ in range(NPASS):
                P.op("dve", I("tensor_scalar", posp[:, p_, :], fl(posm), -float(CAP * p_), None, ALU.add), reads=[B("posm")], writes=[B("posp")])
            P.barrier()

            def moe_sparse():
                TILES = [(0, 512), (512, CAP - 512)]
                xg = Hv(64, 8, CAP)
                Sel = Hv(74, 16, CAP)
                ybf = Hv(74, NB, 1024)
                SelT = Hv(84, 2, NB, 512)
                x_tok = Hv(106, 16, 1024)
                sA = Fv(138, 2, 512)
                sB = Fv(142, 2, 128)
                gBt = Fv(143, 2, 512)
                dgG = Fv(147, 4, 128)
                dgP = Fv(149, 4, 128)
                ring = [Hv(158 + 4 * i, 8, 256) for i in range(6)]
                yacc = Fv(182, NB, 1024)
                actb = Hv(202, 2, 2, CAP)
                dnf = fl(dn)
                petok = lambda: ("pe", P.count["pe"])
                tctr = 0
                for j in range(16):
                    for q in range(2):
                        bank = tctr % 4
                        tctr += 1
                        for kk in range(4):
                            kc = q * 4 + kk
                            mmg(psum[:, bank, kk * 128:(kk + 1) * 128], B("ps", bank), [(xn[:, kc, j * 128:(j + 1) * 128], identb)], [])
                        if tctr % 2:
                            P.op("act", I("activation", x_tok[:, j, q * 512:(q + 1) * 512], PS(bank), AF.Copy), reads=[B("ps", bank)], writes=[B("xtok", j)])
                        else:
                            P.op("dve", I("tensor_copy", x_tok[:, j, q * 512:(q + 1) * 512], PS(bank)), reads=[B("ps", bank)], writes=[B("xtok", j)])
                r1_free = petok()
                rc = [0]
                ac = [0]
                fc = [0]
                dc = [0]
                gc = [0]
                NG = 14
                allg = [(e_, g_) for e_ in range(8) for g_ in range(NG)]

                def load_gu(idx):
                    e_, g_ = allg[idx]
                    f0 = g_ * 2
                    slots = []
                    for t in range(2):
                        si_ = rc[0] % 6
                        rc[0] += 1
                        src = (mg if t == 0 else mu)[e_, g_, :, :]
                        P.dma("pool", ring[si_], src.rearrange("p (kc n) -> p kc n", kc=8), writes=[B("ring", si_)], sembuf=B("grp", idx % 2))
                        slots.append(si_)
                    return slots

                def load_d(idx, slots):
                    e_, g_ = allg[idx]
                    f0 = g_ * 2
                    si_ = rc[0] % 6
                    rc[0] += 1
                    dv = ring[si_].rearrange("p a b -> p (a b)").rearrange("p (f n) -> p f n", n=1024)
                    tok = P.dma("pool", dv, md[e_, g_, :, :].rearrange("p (f n) -> p f n", f=2), writes=[B("ring", si_)], sembuf=B("grp", idx % 2))
                    slots = slots + [si_]
                    for s2 in slots:
                        B("ring", s2).w = tok
                    return slots

                def load_group(idx):
                    return load_d(idx, load_gu(idx))

                tq = [0]
                tmps = [(gBt[:, 0, :], lambda: [B("gBt", 0)]), (gBt[:, 1, :], lambda: [B("gBt", 1)]),
                        (Fv(94, 512), lambda: [B("tmp2")]),
                        (dgG.rearrange("p a b -> p (a b)"), lambda: [B("dgG", jj) for jj in range(4)])]

                def emit_down(wd_, sd, ab, first, last, gather_tok):
                    if last:
                        P.wait_tok("dve", gather_tok)
                    for b in range(NB):
                        for half in range(2):
                            db = 4 + dc[0] % 4
                            dc[0] += 1
                            mmg(PS(db), B("ps", db), [(actb[:, ab, f, b * 128:(b + 1) * 128], wd_[:, f, half * 512:(half + 1) * 512]) for f in range(2)],
                                [B("ring", sd), B("act", ab)])
                            ya = yacc[:, b, half * 512:(half + 1) * 512]
                            if first:
                                P.op("act", I("activation", ya, PS(db), AF.Copy), reads=[B("ps", db)], writes=[B("yacc", b, half)])
                            elif last:
                                P.op("dve", I("tensor_tensor", ybf[:, b, half * 512:(half + 1) * 512], ya, PS(db), ALU.add),
                                     reads=[B("ps", db), B("yacc", b, half)], writes=[B("ybf", b)])
                            elif (b * 2 + half) % 2 == 0:
                                P.op("dve", I("tensor_tensor", ya, ya, PS(db), ALU.add), reads=[B("ps", db), B("yacc", b, half)], writes=[B("yacc", b, half)])
                            else:
                                k = tq[0] % 4
                                tq[0] += 1
                                tap, tbf = tmps[k]
                                tbufs = tbf()
                                P.op("act", I("activation", tap, PS(db), AF.Copy), reads=[B("ps", db)], writes=tbufs)
                                P.op("pool", I("tensor_tensor", ya, ya, tap, ALU.add), reads=tbufs + [B("yacc", b, half)], writes=[B("yacc", b, half)])

                st = {"r1": r1_free, "pend": None}

                def expert_block(e_, p_, chained):
                    posf = posp[:, p_, :]
                    if st["r1"] is not None:
                        P.wait_tok("pool", st["r1"])
                        P.wait_tok("dve", st["r1"])
                    for j in range(16):
                        eng = "pool" if j % 2 else "dve"
                        P.op(eng, I("tensor_scalar", Sel[:, j, :], iota, posf[:, j * 8 + e_:j * 8 + e_ + 1], None, ALU.is_equal),
                             reads=[B("posp"), B("iota")], writes=[B("Sel", j)])
                    for kc in range(8):
                        for (o_, W) in TILES:
                            bank = gc[0] % 4
                            gc[0] += 1
                            jl = list(range(16)) if o_ == 0 else list(range(o_ // 128, 16))
                            mmg(PS(bank, W), B("ps", bank), [(x_tok[:, j, kc * 128:(kc + 1) * 128], Sel[:, j, o_:o_ + W]) for j in jl],
                                [B("xtok", j) for j in jl] + [B("Sel", j) for j in jl])
                            if gc[0] % 2:
                                P.op("act", I("activation", xg[:, kc, o_:o_ + W], PS(bank, W), AF.Copy), reads=[B("ps", bank)], writes=[B("xg", kc)])
                            else:
                                P.op("dve", I("tensor_copy", xg[:, kc, o_:o_ + W], PS(bank, W)), reads=[B("ps", bank)], writes=[B("xg", kc)])
                    gather_tok = petok()
                    pdown = None
                    pend = st["pend"] if (chained and st["pend"] is not None) else load_group(e_ * NG)
                    st["pend"] = None
                    for g_ in range(NG):
                        sg, su, sd = pend
                        nxt = None
                        if g_ + 1 < NG:
                            nxt = e_ * NG + g_ + 1
                        elif chained and e_ + 1 < 8:
                            nxt = (e_ + 1) * NG
                        ngu = load_gu(nxt) if nxt is not None else None
                        wg_, wu_ = ring[sg], ring[su]
                        wd_ = ring[sd].rearrange("p a b -> p (a b)").rearrange("p (f n) -> p f n", n=1024)
                        ab = ac[0] % 2
                        ac[0] += 1
                        for (o_, W) in TILES:
                            for f in range(2):
                                k = fc[0] % 2
                                fc[0] += 1
                                gb_, ub_ = k, 2 + k
                                xr = [B("xg", kc) for kc in range(8)]
                                mmg(PS(gb_, W), B("ps", gb_), [(wg_[:, kc, f * 128:(f + 1) * 128], xg[:, kc, o_:o_ + W]) for kc in range(8)], [B("ring", sg)] + xr)
                                mmg(PS(ub_, W), B("ps", ub_), [(wu_[:, kc, f * 128:(f + 1) * 128], xg[:, kc, o_:o_ + W]) for kc in range(8)], [B("ring", su)] + xr)
                                st_ = sA[:, k, :] if W == 512 else sB[:, k, 0:W]
                                sb_ = B("sA", k) if W == 512 else B("sB", k)
                                P.op("act", I("activation", st_, PS(gb_, W), AF.Silu), reads=[B("ps", gb_)], writes=[sb_])
                                P.op("dve", I("tensor_tensor", actb[:, ab, f, o_:o_ + W], st_, PS(ub_, W), ALU.mult),
                                     reads=[sb_, B("ps", ub_)], writes=[B("act", ab)])
                        if pdown is not None:
                            emit_down(*pdown)
                        pdown = (wd_, sd, ab, g_ == 0, g_ == NG - 1, gather_tok)
                        if nxt is not None:
                            full = load_d(nxt, ngu)
                            if g_ + 1 < NG:
                                pend = full
                            else:
                                st["pend"] = full
                    emit_down(*pdown)
                    for n in range(NT):
                        nb2 = n % 2
                        bx, by = (0, 1) if nb2 == 0 else (2, 3)
                        for jj in range(4):
                            j = n * 4 + jj
                            c_ = j * 8 + e_
                            P.op("dve", I("tensor_scalar", dgG[:, jj, :], ident, dnf[:, c_:c_ + 1], None, ALU.mult), reads=[B("dn"), B("ident")], writes=[B("dgG", jj)])
                            P.op("pool", I("tensor_scalar", dgP[:, jj, :], ident, posf[:, c_:c_ + 1], None, ALU.mult), reads=[B("posp"), B("ident")], writes=[B("dgP", jj)])
                            mmg(psum[:, bx, jj * 128:(jj + 1) * 128], B("ps", bx), [(ones32, dgG[:, jj, :])], [B("ones32"), B("dgG", jj)])
                            mmg(psum[:, by, jj * 128:(jj + 1) * 128], B("ps", by), [(ones32, dgP[:, jj, :])], [B("ones32"), B("dgP", jj)])
                        P.op("act", I("activation", gBt[:, nb2, :], PS(bx), AF.Copy), reads=[B("ps", bx)], writes=[B("gBt", nb2)])
                        bl = [b for b in range(NB) if 128 * b < (n + 1) * 512]
                        for b in bl:
                            P.op("dve", I("scalar_tensor_tensor", SelT[:, nb2, b, :], PS(by), slotcol[:, b:b + 1], gBt[:, nb2, :], ALU.is_equal, ALU.mult),
                                 reads=[B("ps", by), B("gBt", nb2), B("slotcol")], writes=[B("SelT", nb2, b)])
                        for m in range(8):
                            bank = 4 + m % 4
                            mmg(PS(bank), B("ps", bank), [(ybf[:, b, m * 128:(m + 1) * 128], SelT[:, nb2, b, :]) for b in bl],
                                [B("ybf", b) for b in bl] + [B("SelT", nb2, b) for b in bl])
                            P.op("dve", (I("scalar_tensor_tensor", hsrc(m, n), PS(bank), PVC(1, 5, m), hsrc(m, n), ALU.mult, ALU.add)),
                                 reads=[B("ps", bank), hbuf(m, n)], writes=[hbuf(m, n)])
                    st["r1"] = petok()


                for e_ in range(8):
                    expert_block(e_, 0, True)
                for p_ in range(1, NPASS):
                    for e_ in range(8):
                        P.barrier()
                        st["r1"] = None
                        st["pend"] = None
                        P.skip_begin(cnti[0:1, e_:e_ + 1], p_ * CAP + 1)
                        expert_block(e_, p_, False)
                        P.skip_end()

            def moe_dense():
                gB = Fv(114, 2048)
                groups = [(e_, f4 * 4, 4) for e_ in range(8) for f4 in range(7)]
                gstate = {"e": -1}

                def gate_fn(gi, n):
                    e_ = groups[gi][0]
                    if gstate["e"] != e_:
                        gstate["e"] = e_
                        for nn in range(NT):
                            mmg(PS(0), B("ps", 0), [(sel[0:8, e_ * 128:(e_ + 1) * 128], gT[0:8, nn * 512:(nn + 1) * 512])], [B("sel"), B("gT")])
                            P.op("act", (I("activation", gB[:, nn * 512:(nn + 1) * 512], PS(0), AF.Copy)),
                                 reads=[B("ps", 0)], writes=[B("gB", nn)])
                    return gB[:, n * 512:(n + 1) * 512], B("gB", n)

                ffn(1, [(g[1], g[2]) for g in groups],
                    lambda gi: mg[groups[gi][0], :, groups[gi][1] * 128:(groups[gi][1] + 4) * 128],
                    lambda gi: mu[groups[gi][0], :, groups[gi][1] * 128:(groups[gi][1] + 4) * 128],
                    lambda gi: md[groups[gi][0], groups[gi][1] * 128:(groups[gi][1] + 4) * 128, :],
                    gate_fn=gate_fn, ring_base=158, actb_base=122)

            moe_sparse()
            P.barrier()

        for kc in range(8):
            P.dma("sp", yT[kc * 128:(kc + 1) * 128, :], hT[:, kc, :], reads=[B("h", kc, n) for n in range(NT)], writes=[B("out", kc)], sembuf=B("ost", kc % 4))
        for tok in list(P.dma_toks.values()):
            P.wait_tok("sp", tok)
        P.emit()
    return nc


def _fm(v):
    v = np.asarray(v, np.float32)
    return np.ascontiguousarray(v.reshape(-1, 128).T)


def _consts():
    rows = S // 64
    row = np.repeat(np.arange(rows), 64).astype(np.float32)
    col = np.tile(np.arange(64), rows).astype(np.float32)
    inv = (10000.0 ** (-np.arange(16, dtype=np.float32) / 16)).astype(np.float32)
    ar = row[:, None] * inv
    ac = col[:, None] * inv
    ang = np.concatenate([ar, ar, ac, ac], axis=-1)
    cos = np.cos(ang).astype(np.float32).T
    sin = np.sin(ang).astype(np.float32).T
    rope = np.stack([np.concatenate([cos, cos], 0), np.concatenate([sin, sin], 0)], axis=1)
    bones = np.zeros((128, 128), np.float32)
    bones[:64, :64] = 1.0
    bones[64:, 64:] = 1.0
    prot = np.zeros((128, 128), np.float32)
    for base in (0, 64):
        for d in range(64):
            q = d // 16
            if q % 2 == 0:
                prot[base + d + 16, base + d] = -1.0
            else:
                prot[base + d - 16, base + d] = 1.0
    ident = np.eye(128, dtype=np.float32)
    mats = np.ascontiguousarray(np.stack([bones, prot, ident], axis=1))
    sel = np.zeros((8, 8, 128), np.float32)
    for e in range(8):
        sel[e, e, :] = 1.0
    cst2 = np.zeros((128, 776), np.float32)
    cst2[:, 0:128] = np.triu(np.ones((128, 128), np.float32), k=1)
    cst2[:, 128:768] = np.arange(640, dtype=np.float32)[None, :]
    cst2[:, 768:776] = (128.0 * np.arange(8, dtype=np.float32))[None, :] + np.arange(128, dtype=np.float32)[:, None]
    return np.ascontiguousarray(rope), mats, np.ascontiguousarray(sel.reshape(8, 1024)), cst2


_CACHE = {}


def kernel(x, c, ctx, c_ctx, ada_w, ada_b, norm_mix_g, norm_ffn_g,
           w_in_even, q_norm_g, k_norm_g, lam_q1, lam_k1, lam_q2, lam_k2, subln_g,
           conv_w, w_out_even, ffn_w_gate, ffn_w_up, ffn_w_down,
           sgu_w_in, sgu_ln_g, sgu_ln_b, sgu_w_s, sgu_b_s, sgu_w_out,
           router_w, moe_w_gate, moe_w_up, moe_w_down, _upto=4, _nb=None):
    f = lambda a: np.ascontiguousarray(np.asarray(a, dtype=np.float32))
    x = f(x)
    ctx = f(ctx)
    c = f(c)
    rope, mats, sel, cst2 = _consts()
    nb = x.shape[0] if _nb is None else _nb
    shared = {
        "lnb": np.ascontiguousarray(np.broadcast_to(np.concatenate([f(sgu_ln_g)[0], f(sgu_ln_b)[0]])[None, :], (128, 2048))),
        "bs": f(sgu_b_s)[0].reshape(1, 512),
        "rope": rope, "mats": mats, "sel": sel, "cst2": cst2,
        "ada_w": f(ada_w), "w_in": f(w_in_even)[0], "w_out": f(w_out_even)[0],
        "ffn_g": f(ffn_w_gate)[0], "ffn_u": f(ffn_w_up)[0], "ffn_d": f(ffn_w_down)[0],
        "sgu_in": f(sgu_w_in)[0],
        "wsT": np.ascontiguousarray(np.transpose(f(sgu_w_s)[0], (2, 0, 1)).reshape(128, 512)),
        "sgu_out": f(sgu_w_out)[0], "router": f(router_w)[0],
        "moe_g": np.ascontiguousarray(f(moe_w_gate)[0].reshape(8, 8, 128, 14, 256).transpose(0, 3, 2, 1, 4)).reshape(8, 14, 128, 2048),
        "moe_u": np.ascontiguousarray(f(moe_w_up)[0].reshape(8, 8, 128, 14, 256).transpose(0, 3, 2, 1, 4)).reshape(8, 14, 128, 2048),
        "moe_d": np.ascontiguousarray(f(moe_w_down)[0].reshape(8, 14, 2, 128, 1024).transpose(0, 1, 3, 2, 4)).reshape(8, 14, 128, 2048),
    }
    vbase = np.zeros((128, NV), np.float32)
    vbase[:, 8:16] = _fm(c_ctx)
    vbase[:, 16:64] = _fm(f(ada_b)[0])
    vbase[:, 64:112] = _fm(f(ada_b)[1])
    vbase[:, 112:120] = _fm(f(norm_mix_g)[0])
    vbase[:, 120:128] = _fm(f(norm_mix_g)[1])
    vbase[:, 128:136] = _fm(f(norm_ffn_g)[0])
    vbase[:, 136:144] = _fm(f(norm_ffn_g)[1])
    vbase[:, 144] = np.tile(f(q_norm_g)[0], 2)
    vbase[:, 145] = np.tile(f(k_norm_g)[0], 2)
    vbase[:, 146] = f(subln_g)[0]
    cw = f(conv_w)[0]
    for k in range(3):
        vbase[:, 147 + 4 * k:151 + 4 * k] = _fm(cw[k])
    vbase[:, 159] = 1e-6
    for i, lv in enumerate((lam_q1, lam_k1, lam_q2, lam_k2)):
        vbase[:, 160 + 64 * i:224 + 64 * i] = np.broadcast_to(f(lv)[0][None, :], (128, 64))
    in_maps = []
    for b in range(nb):
        v = vbase.copy()
        v[:, 0:8] = _fm(c[b])
        m = dict(shared)
        m["xT"] = np.ascontiguousarray(x[b].T)
        m["ctxT"] = np.ascontiguousarray(ctx[b].T)
        m["vecs"] = v
        in_maps.append(m)
    key = _upto
    if key not in _CACHE:
        _CACHE[key] = build_program(_upto)
    nc = _CACHE[key]
    res = run_bass_kernel_spmd(nc, in_maps, core_ids=list(range(nb)))
    out = np.stack([np.ascontiguousarray(r["yT"].T) for r in res.results], axis=0)
    return out.astype(np.float32)
```

```python
import contextlib
import math
import numpy as np
import concourse.bass as bass
import concourse.mybir as mybir
from concourse.bass_utils import run_bass_kernel_spmd

F32 = mybir.dt.float32
BF16 = mybir.dt.bfloat16
AF = mybir.ActivationFunctionType
ALU = mybir.AluOpType
AX = mybir.AxisListType

D = 1024
S = 2048
LC = 256
NT = 4
NV = 416
LAMBDA_INIT0 = 0.8 - 0.6 * math.exp(0.0)


def I(name, *args, **kw):
    return (name, args, kw)


class Buf:
    __slots__ = ("name", "w", "r", "dsem", "dcount")

    def __init__(self, name):
        self.name = name
        self.w = None
        self.r = {}
        self.dsem = None
        self.dcount = 0


class Prog:
    ENGS = ("pe", "act", "dve", "pool", "sp")

    def __init__(self, nc, stack):
        self.nc = nc
        self.stack = stack
        self.sem = {}
        self.count = {}
        self.seen = {e: {} for e in self.ENGS}
        self.q = {e: [] for e in self.ENGS}
        self.pending = {e: False for e in self.ENGS}
        for e in self.ENGS:
            self.sem[e] = stack.enter_context(nc.semaphore("prog_" + e))
            self.count[e] = 0
        self.bufs = {}
        self.dma_bufs = {}
        self.dma_toks = {}
        self.dma_free = {}
        self.dma_all = []
        self.epoch = 0
        self.maxcount = 0

    def B(self, *key):
        b = self.bufs.get(key)
        if b is None:
            b = Buf("_".join(str(k) for k in key))
            self.bufs[key] = b
        return b

    def _dsem(self, b, eng):
        key = (eng, b.name)
        ent = self.dma_bufs.get(key)
        if ent is None:
            free = self.dma_free.setdefault(eng, [])
            if free:
                ent = free.pop()
            else:
                self.ndsem = getattr(self, "ndsem", 0) + 1
                ent = [self.stack.enter_context(self.nc.semaphore("dma_%s_%d" % (eng, self.ndsem))), 0]
                self.dma_all.append((eng, ent))
            self.dma_bufs[key] = ent
        return ent

    def _waits(self, eng, reads, writes):
        need = {}

        def add(tok):
            if tok is None:
                return
            k, v = tok
            if need.get(k, 0) < v:
                need[k] = v

        for b in reads:
            add(b.w)
        for b in writes:
            add(b.w)
            for k, v in b.r.items():
                add((k, v))
        out = []
        for k, v in need.items():
            if k == eng and eng == "pe":
                continue
            if self.seen[eng].get(k, 0) >= v:
                continue
            self.seen[eng][k] = v
            out.append((k, v))
        return out

    def _semof(self, k):
        return self.sem[k] if isinstance(k, str) else k

    def op(self, eng, fn, reads=(), writes=(), inc=True):
        for k, v in self._waits(eng, reads, writes):
            self.q[eng].append(("wait", self._semof(k), v))
        if inc:
            self.count[eng] += 1
            tok = (eng, self.count[eng])
            self.pending[eng] = False
        else:
            tok = (eng, self.count[eng] + 1)
            self.pending[eng] = True
        self.q[eng].append(("op", fn, inc, self.sem[eng]))
        for b in reads:
            if b.r.get(eng, 0) < tok[1]:
                b.r[eng] = tok[1]
        for b in writes:
            b.w = tok
            b.r = {}
        return tok

    def dma(self, eng, out_ap, in_ap, reads=(), writes=(), sembuf=None):
        sb = sembuf if sembuf is not None else (writes[0] if writes else reads[0])
        ent = self._dsem(sb, eng)
        for k, v in self._waits(eng, reads, writes):
            self.q[eng].append(("wait", self._semof(k), v))
        ent[1] += 16
        tok = (ent[0], ent[1])
        self.dma_toks[id(ent[0])] = tok
        self.q[eng].append(("dma", out_ap, in_ap, ent[0]))
        for b in reads:
            if b.r.get(tok[0], 0) < tok[1]:
                b.r[tok[0]] = tok[1]
        for b in writes:
            b.w = tok
            b.r = {}
        return tok

    def wait_tok(self, eng, tok):
        k, v = tok
        if self.seen[eng].get(k, 0) >= v:
            return
        self.seen[eng][k] = v
        self.q[eng].append(("wait", self._semof(k), v))

    def barrier(self):
        for e in self.ENGS:
            assert not self.pending[e], e
        for e in self.ENGS:
            for o in self.ENGS:
                if self.count[o] > 0:
                    self.wait_tok(e, (o, self.count[o]))
            for tok in self.dma_toks.values():
                self.wait_tok(e, tok)
        self.bufs = {}
        self.epoch += 1
        self.dma_bufs = {}
        self.dma_free = {}
        for eng, ent in self.dma_all:
            self.dma_free.setdefault(eng, []).append(ent)

    def fresh(self):
        self.nfresh = getattr(self, "nfresh", 0) + 1
        for e in self.ENGS:
            self.sem[e] = self.stack.enter_context(self.nc.semaphore("prog_%s_f%d" % (e, self.nfresh)))
            self.count[e] = 0
            self.seen[e] = {}
        self.dma_bufs = {}
        self.dma_free = {}
        self.dma_all = []
        self.dma_toks = {}
        self.bufs = {}

    def dyn_if(self, cond_ap, thr):
        for e in self.ENGS:
            self.q[e].append(("if", cond_ap, thr))

    def skip_begin(self, cond_ap, thr):
        self._skip = {"count": dict(self.count), "ndma": len(self.dma_all),
                      "bump": {e: [] for e in self.ENGS}}
        self.dma_free = {}
        self.dma_bufs = {}
        for e in self.ENGS:
            self.q[e].append(("if", cond_ap, thr))
            self.q[e].append(("bump", self._skip["bump"][e], self.sem[e], self.count[e]))
            self.q[e].append(("else",))

    def skip_end(self):
        sk = self._skip
        for e in self.ENGS:
            d = self.count[e] - sk["count"][e]
            if d:
                sk["bump"][e].append((self.sem[e], d))
        for eng, ent in self.dma_all[sk["ndma"]:]:
            self.dma_toks.pop(id(ent[0]), None)
        del self.dma_all[sk["ndma"]:]
        self.dma_bufs = {}
        for e in self.ENGS:
            self.q[e].append(("endif",))

    def dyn_else(self):
        for e in self.ENGS:
            self.q[e].append(("else",))

    def dyn_end(self):
        for e in self.ENGS:
            self.q[e].append(("endif",))

    def emit(self):
        nc = self.nc
        for e in self.ENGS:
            assert not self.pending[e], f"engine {e} has trailing un-inc'd op"
        engobj = {"pe": "tensor", "act": "scalar", "dve": "vector", "pool": "gpsimd", "sp": "sync"}
        with nc.Block() as block:
            for e in self.ENGS:
                items = self.q[e]
                sem = self.sem[e]

                def body(engine, items=items, sem=sem):
                    guards = []
                    nreg = [0]
                    for it in items:
                        if it[0] == "if":
                            nreg[0] += 1
                            rg = engine.register("dynr%d" % nreg[0])
                            r = rg.__enter__()
                            engine.load(r, it[1], bass_reorder=False)
                            g = engine.If_lt(r, it[2])
                            g.__enter__()
                            guards.append(g)
                            guards.append(rg)
                        elif it[0] == "bump":
                            if it[3] > 0:
                                engine.wait_ge(it[2], it[3])
                            for bsem, amt in it[1]:
                                while amt > 0:
                                    a = min(amt, 4096)
                                    engine.sem_inc(bsem, a)
                                    amt -= a
                        elif it[0] == "else":
                            rg = guards.pop()
                            guards.pop().__exit__(None, None, None)
                            g = engine.Else()
                            g.__enter__()
                            guards.append(g)
                            guards.append(rg)
                        elif it[0] == "endif":
                            rg = guards.pop()
                            guards.pop().__exit__(None, None, None)
                            rg.__exit__(None, None, None)
                        elif it[0] == "wait":
                            engine.wait_ge(it[1], it[2])
                        elif it[0] == "op":
                            name, args, kw = it[1]
                            ins = getattr(engine, name)(*args, **kw)
                            if it[2]:
                                ins.then_inc(it[3], 1)
                        else:
                            engine.dma_start(out=it[1], in_=it[2]).then_inc(it[3], 16)

                getattr(block, engobj[e])(body)


def build_program(upto=4):
    nc = bass.Bass("TRN2", target_bir_lowering=False)

    def din(name, shape):
        return nc.dram_tensor(name, list(shape), F32, kind="ExternalInput").ap()

    xT = din("xT", [D, S])
    ctxT = din("ctxT", [D, LC])
    vecs = din("vecs", [128, NV])
    lnb = din("lnb", [128, 2048])
    bsd = din("bs", [1, 512])
    rope = din("rope", [128, 2, S])
    mats = din("mats", [128, 3, 128])
    seld = din("sel", [8, 1024])
    cst2 = din("cst2", [128, 776])
    ada_w = din("ada_w", [2, D, 6 * D])
    w_in = din("w_in", [D, 3072])
    w_out = din("w_out", [D, D])
    ffg = din("ffn_g", [D, 2816])
    ffu = din("ffn_u", [D, 2816])
    ffd = din("ffn_d", [2816, D])
    sgi = din("sgu_in", [D, 2048])
    wsTd = din("wsT", [128, 512])
    sgo = din("sgu_out", [D, D])
    rtr = din("router", [D, 8])
    mg = din("moe_g", [8, 14, 128, 2048])
    mu = din("moe_u", [8, 14, 128, 2048])
    md = din("moe_d", [8, 14, 128, 2048])
    yT = nc.dram_tensor("yT", [D, S], F32, kind="ExternalOutput").ap()

    with contextlib.ExitStack() as st:
        P = Prog(nc, st)
        B = P.B
        AW = 207 * 256
        arena = st.enter_context(nc.sbuf_tensor("arena", [128, AW], F32))
        psum = st.enter_context(nc.psum_tensor("psum", [128, 8, 512], F32))

        def Fv(kib, *shape):
            n = int(np.prod(shape))
            w0 = int(round(kib * 256))
            v = arena[:, w0:w0 + n]
            if len(shape) == 2:
                return v.rearrange("p (a b) -> p a b", a=shape[0])
            if len(shape) == 3:
                return v.rearrange("p (a b c) -> p a b c", a=shape[0], b=shape[1])
            return v

        def Hv(kib, *shape):
            n = int(np.prod(shape))
            assert n % 2 == 0
            w0 = int(round(kib * 256))
            v = arena[:, w0:w0 + n // 2].bitcast(BF16)
            if len(shape) == 2:
                return v.rearrange("p (a b) -> p a b", a=shape[0])
            if len(shape) == 3:
                return v.rearrange("p (a b c) -> p a b c", a=shape[0], b=shape[1])
            return v

        def PS(b, w=512):
            return psum[:, b, 0:w]

        hT = Fv(0, 8, S)
        xn = Hv(64, 8, S)
        xc = Hv(96, 8, LC)
        vt = Fv(100, NV)
        pv = Fv(101.75, 128)
        ones = Hv(102.25, 128)
        bones = Hv(102.5, 128)
        prot = Hv(102.75, 128)
        ident = Fv(103, 128)
        mod = Fv(103.5, 2, 96)
        scb = Hv(104.25, 8, 2)
        lamt = Fv(104.5, 64)
        eps = vt[:, 159:160]

        def PVC(layer, which, kc):
            return pv[:, layer * 48 + which * 8 + kc: layer * 48 + which * 8 + kc + 1]
        NEG_LAM = pv[:, 112:113]
        GSUB = pv[:, 113:114]

        def mmg(out_ap, out_buf, terms, reads):
            n = len(terms)
            for i, (l, r) in enumerate(terms):
                P.op("pe", (I("matmul", out_ap, l, r, start=(i == 0), stop=(i == n - 1))),
                     reads=reads, writes=[out_buf], inc=(i == n - 1))

        def slab_load(dst, src_rows, bufkey):
            P.dma("pool", dst, src_rows.rearrange("(kc p) n -> p kc n", p=128), writes=[bufkey])

        MT = 190

        def modulate(src_fn, src_buf, W, ntiles, dst_fn, dst_buf, gs_fn, s_fn, f32_fn=None, f32_buf=None):
            sq = Hv(MT, 2, 512)
            rt = Fv(MT + 2, 512)
            tt = Fv(MT + 4, 2, 512)
            for n in range(ntiles):
                for kc in range(8):
                    sb = sq[:, kc % 2, 0:W]
                    P.op("act", (I("activation", sb, src_fn(kc, n), AF.Square)),
                         reads=[src_buf(kc, n)], writes=[B("msq", kc % 2)])
                    P.op("pe", (I("matmul", PS(7, W), ones, sb, start=(kc == 0), stop=(kc == 7))),
                         reads=[B("msq", kc % 2), B("ones")], writes=[B("ps", 7)])
                P.op("act", (I("activation", rt[:, 0:W], PS(7, W), AF.Sqrt, bias=eps, scale=1.0 / D)),
                     reads=[B("ps", 7), B("vt")], writes=[B("mrt")])
                P.op("dve", (I("reciprocal", rt[:, 0:W], rt[:, 0:W])), reads=[B("mrt")], writes=[B("mrt")])
                for kc in range(8):
                    tb = tt[:, kc % 2, 0:W]
                    P.op("dve", (I("scalar_tensor_tensor", tb, src_fn(kc, n), gs_fn(kc), rt[:, 0:W], ALU.mult, ALU.mult)),
                         reads=[src_buf(kc, n), B("mrt"), B("pv")], writes=[B("mt", kc % 2)])
                    if f32_fn is None:
                        P.op("act", (I("activation", dst_fn(kc, n), tb, AF.Identity, bias=s_fn(kc))),
                             reads=[B("mt", kc % 2), B("pv")], writes=[dst_buf(kc, n)])
                    else:
                        P.op("act", (I("activation", f32_fn(kc, n), tb, AF.Identity, bias=s_fn(kc))),
                             reads=[B("mt", kc % 2), B("pv")], writes=[f32_buf(kc, n)])
                        P.op("dve", (I("tensor_copy", dst_fn(kc, n), f32_fn(kc, n))),
                             reads=[f32_buf(kc, n)], writes=[dst_buf(kc, n)])

        def hsrc(kc, n):
            return hT[:, kc, n * 512:(n + 1) * 512]

        def hbuf(kc, n):
            return B("h", kc, n)

        def xdst(kc, n):
            return xn[:, kc, n * 512:(n + 1) * 512]

        def xbuf(kc, n):
            return B("xn", kc, n)

        P.dma("sp", vt, vecs, writes=[B("vt")])
        for kc in range(8):
            P.dma("sp", hT[:, kc, :], xT[kc * 128:(kc + 1) * 128, :], writes=[B("h", kc, n) for n in range(NT)], sembuf=B("hload", kc % 4))
        P.dma("sp", ident, mats[:, 2, :], writes=[B("ident")])
        P.dma("pool", bones, mats[:, 0, :], writes=[B("bones")])
        P.dma("pool", prot, mats[:, 1, :], writes=[B("prot")])
        P.op("pool", I("memset", ones, 1.0), writes=[B("ones")])
        P.op("act", I("activation", scb[:, :, 0], vt[:, 0:8], AF.Silu), reads=[B("vt")], writes=[B("scb")])
        P.op("act", I("activation", scb[:, :, 1], vt[:, 8:16], AF.Silu), reads=[B("vt")], writes=[B("scb")])
        ring0 = [Hv(106 + 8 * i, 8, 512) for i in range(4)]

        def ada_dma(layer, s, slot, sbuf):
            slab_load(slot, ada_w[layer, :, s * 512:(s + 1) * 512], sbuf)

        def ada_mm(layer, s, slot, sbuf, bank):
            for mm in range(4):
                m = s * 4 + mm
                mmg(psum[:, bank, m * 2:m * 2 + 2], B("ps", bank),
                    [(slot[:, kc, mm * 128:(mm + 1) * 128], scb[:, kc, :]) for kc in range(8)],
                    [sbuf, B("scb")])

        def ada_add(layer, bank):
            ab = vt[:, 16 + 48 * layer:64 + 48 * layer]
            for col in range(2):
                P.op("dve", (I("tensor_tensor",
                    mod[:, layer, :].rearrange("p (m c) -> p m c", c=2)[:, :, col],
                    psum[:, bank, 0:96].rearrange("p (m c) -> p m c", c=2)[:, :, col], ab, ALU.add)),
                    reads=[B("ps", bank), B("vt")], writes=[B("mod", layer)])

        for s in range(12):
            ada_dma(0, s, ring0[s % 4], B("r0", s % 4))
            ada_mm(0, s, ring0[s % 4], B("r0", s % 4), 0)
        ada_add(0, 0)

        def modv(layer, chunk, col=0):
            return mod[:, layer, :].rearrange("p (m c) -> p m c", c=2)[:, chunk * 8:(chunk + 1) * 8, col]

        def ada_derive(layer):
            nmg = vt[:, 112 + 8 * layer:120 + 8 * layer]
            nfg = vt[:, 128 + 8 * layer:136 + 8 * layer]
            b0 = layer * 48
            ops = [
                (I("scalar_tensor_tensor", pv[:, b0:b0 + 8], modv(layer, 1), 1.0, nmg, ALU.add, ALU.mult)),
                (I("tensor_copy", pv[:, b0 + 8:b0 + 16], modv(layer, 0))),
                (I("tensor_copy", pv[:, b0 + 16:b0 + 24], modv(layer, 2))),
                (I("scalar_tensor_tensor", pv[:, b0 + 24:b0 + 32], modv(layer, 4), 1.0, nfg, ALU.add, ALU.mult)),
                (I("tensor_copy", pv[:, b0 + 32:b0 + 40], modv(layer, 3))),
                (I("tensor_copy", pv[:, b0 + 40:b0 + 48], modv(layer, 5))),
            ]
            for f in ops:
                P.op("dve", f, reads=[B("mod", layer), B("vt")], writes=[B("pv")])

        ada_derive(0)
        P.op("dve", I("scalar_tensor_tensor", pv[:, 96:104], modv(0, 1, 1), 1.0, vt[:, 112:120], ALU.add, ALU.mult),
             reads=[B("mod", 0), B("vt")], writes=[B("pv")])
        P.op("dve", I("tensor_copy", pv[:, 104:112], modv(0, 0, 1)), reads=[B("mod", 0)], writes=[B("pv")])
        P.op("dve", I("tensor_tensor", lamt[:, 0:64], vt[:, 160:224], vt[:, 224:288], ALU.mult), reads=[B("vt")], writes=[B("lamt")])
        P.op("dve", I("reduce_sum", pv[:, 114:115], lamt[:, 0:64], AX.X), reads=[B("lamt")], writes=[B("pv")])
        P.op("dve", I("tensor_tensor", lamt[:, 0:64], vt[:, 288:352], vt[:, 352:416], ALU.mult), reads=[B("vt"), B("pv")], writes=[B("lamt")])
        P.op("dve", I("reduce_sum", pv[:, 115:116], lamt[:, 0:64], AX.X), reads=[B("lamt")], writes=[B("pv")])
        P.op("act", I("activation", pv[:, 116:118], pv[:, 114:116], AF.Exp), reads=[B("pv")], writes=[B("pv2")])
        P.op("dve", I("tensor_tensor", pv[:, 118:119], pv[:, 117:118], pv[:, 116:117], ALU.subtract), reads=[B("pv2")], writes=[B("pv3")])
        P.op("dve", I("tensor_scalar", NEG_LAM, pv[:, 118:119], -LAMBDA_INIT0, None, ALU.add), reads=[B("pv3")], writes=[B("pv")])
        P.op("dve", I("tensor_scalar", GSUB, vt[:, 146:147], 1.0 - LAMBDA_INIT0, None, ALU.mult), reads=[B("vt")], writes=[B("pv")])
        P.barrier()

        if upto >= 1:
            modulate(hsrc, hbuf, 512, NT, xdst, xbuf, lambda kc: PVC(0, 0, kc), lambda kc: PVC(0, 1, kc))
            c32 = Fv(106, 8, LC)
            for kc in range(8):
                P.dma("sp", c32[:, kc, :], ctxT[kc * 128:(kc + 1) * 128, :], writes=[B("c32", kc)], sembuf=B("c32ld"))
            for kc in range(8):
                B("c32", kc).w = B("c32", 7).w
            modulate(lambda kc, n: c32[:, kc, :], lambda kc, n: B("c32", kc), LC, 1,
                     lambda kc, n: xc[:, kc, :], lambda kc, n: B("xc", kc),
                     lambda kc: pv[:, 96 + kc:97 + kc], lambda kc: pv[:, 104 + kc:105 + kc])
            P.barrier()

            qT = Hv(106, 4, S)
            kT = Hv(122, 4, S + LC)
            vtok = Hv(140, 18, 512)
            wq = Hv(158, 8, 512)
            wk = Hv(166, 8, 512)
            wv = Hv(174, 8, 512)
            cs = Fv(182, 2, 2, 512)
            qg = Hv(190, 2, 512)
            sqq = Hv(192, 2, 512)
            rtq = Fv(194, 512)
            t1 = Fv(196, 512)
            t2 = Fv(198, 512)
            slab_load(wk, w_in[:, 512:1024], B("wk"))
            slab_load(wv, w_in[:, 1024:1536], B("wv"))
            slab_load(wq, w_in[:, 0:512], B("wq"))
            ectr = [0]

            def qk_epilogue(bank, W, gcol, dst, dst_buf, n, use_rope):
                i = ectr[0] % 2
                ectr[0] += 1
                A = PS(bank, W)
                P.op("act", (I("activation", qg[:, i, 0:W], A, AF.Identity, scale=vt[:, gcol:gcol + 1])),
                     reads=[B("ps", bank), B("vt")], writes=[B("qg", i)])
                P.op("act", (I("activation", sqq[:, i, 0:W], A, AF.Square)),
                     reads=[B("ps", bank)], writes=[B("sqq", i)])
                bs_, bc_ = 2 + i, 4 + i
                mmg(PS(bs_, W), B("ps", bs_), [(bones, sqq[:, i, 0:W])], [B("bones"), B("sqq", i)])
                if use_rope:
                    mmg(PS(bc_, W), B("ps", bc_), [(prot, qg[:, i, 0:W])], [B("prot"), B("qg", i)])
                P.op("act", (I("activation", rtq[:, 0:W], PS(bs_, W), AF.Sqrt, bias=eps, scale=1.0 / 64)),
                     reads=[B("ps", bs_), B("vt")], writes=[B("rtq")])
                P.op("dve", (I("reciprocal", rtq[:, 0:W], rtq[:, 0:W])), reads=[B("rtq")], writes=[B("rtq")])
                if use_rope:
                    cb = n % 2
                    P.op("dve", (I("tensor_tensor", t1[:, 0:W], qg[:, i, 0:W], cs[:, cb, 0, :], ALU.mult)),
                         reads=[B("qg", i), B("cs", cb)], writes=[B("t1")])
                    P.op("dve", (I("tensor_tensor", t2[:, 0:W], PS(bc_, W), cs[:, cb, 1, :], ALU.mult)),
                         reads=[B("ps", bc_), B("cs", cb)], writes=[B("t2")])
                    P.op("dve", (I("tensor_tensor", t1[:, 0:W], t1[:, 0:W], t2[:, 0:W], ALU.add)),
                         reads=[B("t1"), B("t2")], writes=[B("t1")])
                    P.op("dve", (I("tensor_tensor", dst, t1[:, 0:W], rtq[:, 0:W], ALU.mult)),
                         reads=[B("t1"), B("rtq")], writes=[dst_buf])
                else:
                    P.op("dve", (I("tensor_tensor", dst, qg[:, i, 0:W], rtq[:, 0:W], ALU.mult)),
                         reads=[B("qg", i), B("rtq")], writes=[dst_buf])

            actr = [0]
            for hd in range(4):
                bank = actr[0] % 2
                actr[0] += 1
                mmg(PS(bank, LC), B("ps", bank), [(wk[:, kc, hd * 128:(hd + 1) * 128], xc[:, kc, :]) for kc in range(8)],
                    [B("wk")] + [B("xc", kc) for kc in range(8)])
                qk_epilogue(bank, LC, 145, kT[:, hd, 0:LC], B("kT", hd, "c"), 0, False)
            vctr = [0]

            def v_chunk(src_fn, src_bufs, j):
                bank = 6 + vctr[0] % 2
                vctr[0] += 1
                mmg(PS(bank), B("ps", bank), [(src_fn(kc), wv[:, kc, :]) for kc in range(8)], [B("wv")] + src_bufs)
                P.op("act", (I("activation", vtok[:, j, :], PS(bank), AF.Copy)), reads=[B("ps", bank)], writes=[B("vtok", j)])

            for j in range(2):
                v_chunk(lambda kc, j=j: xc[:, kc, j * 128:(j + 1) * 128], [B("xc", kc) for kc in range(8)], j)
            for n in range(NT):
                cb = n % 2
                P.dma("sp", cs[:, cb, :, :], rope[:, :, n * 512:(n + 1) * 512], writes=[B("cs", cb)])
                for which in range(2):
                    w_ = wk if which == 0 else wq
                    wb = B("wk") if which == 0 else B("wq")
                    for hd in range(4):
                        bank = actr[0] % 2
                        actr[0] += 1
                        mmg(PS(bank), B("ps", bank), [(w_[:, kc, hd * 128:(hd + 1) * 128], xdst(kc, n)) for kc in range(8)],
                            [wb] + [xbuf(kc, n) for kc in range(8)])
                        if which == 0:
                            qk_epilogue(bank, 512, 145, kT[:, hd, LC + n * 512:LC + (n + 1) * 512], B("kT", hd, n), n, True)
                        else:
                            qk_epilogue(bank, 512, 144, qT[:, hd, n * 512:(n + 1) * 512], B("qT", hd, n), n, True)
                for jj in range(4):
                    j = n * 4 + jj
                    v_chunk(lambda kc, j=j: xn[:, kc, j * 128:(j + 1) * 128], [xbuf(kc, n) for kc in range(8)], 2 + j)
            P.barrier()

            yA = Hv(158, 4, S)
            pT = Hv(174, 4, 512)
            r0 = Fv(178, 512)
            r1 = Fv(180, 512)
            aa = Fv(182, 512)
            bb = Fv(184, 512)
            sqa = Hv(186, 512)
            NJ = 18
            for n in range(NT):
                for hd in range(4):
                    steps = [(c, j) for c in range(2) for j in range(NJ)]

                    def qk(i):
                        c, j = steps[i]
                        sb_ = i % 3
                        mmg(PS(sb_), B("ps", sb_),
                            [(kT[c * 64:(c + 1) * 64, hd, j * 128:(j + 1) * 128], qT[c * 64:(c + 1) * 64, hd, n * 512:(n + 1) * 512])], [])
                        P.op("act", (I("activation", pT[:, i % 4, :], PS(sb_), AF.Exp, scale=0.125)),
                             reads=[B("ps", sb_)], writes=[B("pT", i % 4)])

                    def pvz(i):
                        c, j = steps[i]
                        ob, zb = 3 + c, 5 + c
                        P.op("pe", (I("matmul", PS(ob), vtok[:, j, hd * 128:(hd + 1) * 128], pT[:, i % 4, :], start=(j == 0), stop=(j == NJ - 1))),
                             reads=[B("pT", i % 4)], writes=[B("ps", ob)], inc=False)
                        P.op("pe", (I("matmul", PS(zb), ones, pT[:, i % 4, :], start=(j == 0), stop=(j == NJ - 1))),
                             reads=[B("pT", i % 4)], writes=[B("ps", zb)], inc=True)

                    qk(0)
                    qk(1)
                    for i in range(len(steps)):
                        pvz(i)
                        if i + 2 < len(steps):
                            qk(i + 2)
                    P.op("dve", I("reciprocal", r0, PS(5)), reads=[B("ps", 5)], writes=[B("r0")])
                    P.op("dve", I("reciprocal", r1, PS(6)), reads=[B("ps", 6)], writes=[B("r1")])
                    P.op("dve", I("tensor_tensor", aa, PS(3), r0, ALU.mult), reads=[B("ps", 3), B("r0")], writes=[B("aa")])
                    P.op("dve", I("tensor_tensor", bb, PS(4), r1, ALU.mult), reads=[B("ps", 4), B("r1")], writes=[B("bb")])
                    P.op("dve", I("scalar_tensor_tensor", aa, bb, NEG_LAM, aa, ALU.mult, ALU.add), reads=[B("aa"), B("bb")], writes=[B("aa")])
                    P.op("act", I("activation", sqa, aa, AF.Square), reads=[B("aa")], writes=[B("sqa")])
                    mmg(PS(7), B("ps", 7), [(ones, sqa)], [B("sqa")])
                    P.op("act", I("activation", r0, PS(7), AF.Sqrt, bias=eps, scale=1.0 / 128), reads=[B("ps", 7)], writes=[B("r0")])
                    P.op("dve", I("reciprocal", r0, r0), reads=[B("r0")], writes=[B("r0")])
                    P.op("dve", (I("scalar_tensor_tensor", yA[:, hd, n * 512:(n + 1) * 512], aa, GSUB, r0, ALU.mult, ALU.mult)),
                         reads=[B("aa"), B("r0")], writes=[B("yA", hd, n)])
            P.barrier()

            yC = Hv(106, 4, S)
            cslab = [Hv(122 + 6 * i, 8, 384) for i in range(2)]
            g3 = Hv(134, 3, S)
            zz = Fv(146, 2050)
            acc = Fv(174, S)
            P.op("pool", I("memset", zz[:, 0:1], 0.0), writes=[B("zz")])
            P.op("pool", I("memset", zz[:, 2049:2050], 0.0), writes=[B("zz")])
            cctr = 0
            for c in range(4):
                sl = cslab[c % 2]
                for t3 in range(3):
                    col = 1536 + 512 * t3 + c * 128
                    P.dma("pool", sl[:, :, t3 * 128:(t3 + 1) * 128], w_in[:, col:col + 128].rearrange("(kc p) n -> p kc n", p=128),
                          writes=[B("cslab", c % 2)])
                for t3 in range(3):
                    for n in range(NT):
                        bank = cctr % 4
                        cctr += 1
                        mmg(PS(bank), B("ps", bank), [(sl[:, kc, t3 * 128:(t3 + 1) * 128], xdst(kc, n)) for kc in range(8)], [B("cslab", c % 2)])
                        P.op("act", (I("activation", g3[:, t3, n * 512:(n + 1) * 512], PS(bank), AF.Copy)),
                             reads=[B("ps", bank)], writes=[B("g3", t3)])
                w0 = vt[:, 147 + c:148 + c]
                w1 = vt[:, 151 + c:152 + c]
                w2 = vt[:, 155 + c:156 + c]
                P.op("dve", I("tensor_tensor", zz[:, 1:2049], g3[:, 1, :], g3[:, 2, :], ALU.mult), reads=[B("g3", 1), B("g3", 2)], writes=[B("zz")])
                P.op("dve", (I("tensor_scalar", acc, zz[:, 1:2049], w1, None, ALU.mult)), reads=[B("zz")], writes=[B("acc")])
                P.op("dve", (I("scalar_tensor_tensor", acc, zz[:, 0:2048], w0, acc, ALU.mult, ALU.add)), reads=[B("zz"), B("acc")], writes=[B("acc")])
                P.op("dve", (I("scalar_tensor_tensor", acc, zz[:, 2:2050], w2, acc, ALU.mult, ALU.add)), reads=[B("zz"), B("acc")], writes=[B("acc")])
                P.op("dve", (I("tensor_tensor", yC[:, c, :], acc, g3[:, 0, :], ALU.mult)), reads=[B("acc"), B("g3", 0)], writes=[B("yC", c)])
            P.barrier()

            wo = [Hv(122 + 8 * i, 8, 512) for i in range(2)]
            for s in range(2):
                slab_load(wo[s], w_out[:, s * 512:(s + 1) * 512], B("wo", s))
            octr = 0
            for m in range(8):
                for n in range(NT):
                    bank = octr % 4
                    octr += 1
                    terms = []
                    for kc in range(8):
                        ysrc = yA[:, kc, n * 512:(n + 1) * 512] if kc < 4 else yC[:, kc - 4, n * 512:(n + 1) * 512]
                        terms.append((wo[m // 4][:, kc, (m % 4) * 128:(m % 4 + 1) * 128], ysrc))
                    mmg(PS(bank), B("ps", bank), terms, [B("wo", m // 4)])
                    P.op("dve", (I("scalar_tensor_tensor", hsrc(m, n), PS(bank), PVC(0, 2, m), hsrc(m, n), ALU.mult, ALU.add)),
                         reads=[B("ps", bank), hbuf(m, n)], writes=[hbuf(m, n)])
            P.barrier()

        def ffn(layer, groups, gate_src, up_src, down_src, gate_fn=None, hook=None, dbanks=4, ring_base=138, actb_base=198):
            ring = [Hv(ring_base + 8 * i, 8, 512) for i in range(6)]
            actb = Hv(actb_base, 2, 4, 512)
            sS = Fv(106, 2, 512)
            sT = Fv(110, 2, 512)
            rc = [0]
            ac = [0]
            fc = [0]
            dc = [0]

            def load_group(gi):
                f0, nf = groups[gi]
                slots = []
                for t in range(3):
                    si_ = rc[0] % 6
                    rc[0] += 1
                    if t < 2:
                        src = (gate_src if t == 0 else up_src)(gi)
                        P.dma("pool", ring[si_][:, :, 0:nf * 128], src.rearrange("(kc p) n -> p kc n", p=128), writes=[B("ring", si_)])
                    else:
                        dv = ring[si_].rearrange("p a b -> p (a b)")[:, 0:nf * 1024].rearrange("p (f n) -> p f n", n=1024)
                        P.dma("pool", dv, down_src(gi).rearrange("(f p) n -> p f n", p=128), writes=[B("ring", si_)])
                    slots.append(si_)
                return slots

            def emit_down(wd_, sd, nf, ab, n):
                for m in range(8):
                    db = 4 + dc[0] % dbanks
                    dc[0] += 1
                    mmg(PS(db), B("ps", db), [(wd_[:, f, m * 128:(m + 1) * 128], actb[:, ab, f, :]) for f in range(nf)],
                        [B("ring", sd)] + [B("act", ab, f) for f in range(nf)])
                    P.op("dve", (I("scalar_tensor_tensor", hsrc(m, n), PS(db), PVC(layer, 5, m), hsrc(m, n), ALU.mult, ALU.add)),
                         reads=[B("ps", db), hbuf(m, n)], writes=[hbuf(m, n)])

            pend = load_group(0)
            pdown = None
            for gi in range(len(groups)):
                f0, nf = groups[gi]
                sg, su, sd = pend
                wg_, wu_ = ring[sg], ring[su]
                wd_ = ring[sd].rearrange("p a b -> p (a b)")[:, 0:nf * 1024].rearrange("p (f n) -> p f n", n=1024)
                for n in range(NT):
                    ab = ac[0] % 2
                    ac[0] += 1
                    if gate_fn is not None:
                        gap, gbuf = gate_fn(gi, n)
                    for f in range(nf):
                        k = fc[0] % 2
                        fc[0] += 1
                        gb_, ub_ = k, 2 + k
                        mmg(PS(gb_), B("ps", gb_), [(wg_[:, kc, f * 128:(f + 1) * 128], xdst(kc, n)) for kc in range(8)],
                            [B("ring", sg)] + [xbuf(kc, n) for kc in range(8)])
                        mmg(PS(ub_), B("ps", ub_), [(wu_[:, kc, f * 128:(f + 1) * 128], xdst(kc, n)) for kc in range(8)],
                            [B("ring", su)] + [xbuf(kc, n) for kc in range(8)])
                        P.op("act", (I("activation", sS[:, k, :], PS(gb_), AF.Silu)),
                             reads=[B("ps", gb_)], writes=[B("sS", k)])
                        if gate_fn is None:
                            P.op("dve", (I("tensor_tensor", actb[:, ab, f, :], sS[:, k, :], PS(ub_), ALU.mult)),
                                 reads=[B("sS", k), B("ps", ub_)], writes=[B("act", ab, f)])
                        else:
                            P.op("dve", (I("tensor_tensor", sT[:, k, :], sS[:, k, :], PS(ub_), ALU.mult)),
                                 reads=[B("sS", k), B("ps", ub_)], writes=[B("sT", k)])
                            P.op("pool", (I("tensor_tensor", actb[:, ab, f, :], sT[:, k, :], gap, ALU.mult)),
                                 reads=[B("sT", k), gbuf], writes=[B("act", ab, f)])
                    if pdown is not None:
                        emit_down(*pdown)
                    pdown = (wd_, sd, nf, ab, n)
                    if n == 0 and gi + 1 < len(groups):
                        pend = load_group(gi + 1)
                    if hook is not None:
                        hook(gi * NT + n)
            emit_down(*pdown)

        if upto >= 2:
            modulate(hsrc, hbuf, 512, NT, xdst, xbuf, lambda kc: PVC(0, 3, kc), lambda kc: PVC(0, 4, kc))
            groups = [(0, 4), (4, 4), (8, 4), (12, 4), (16, 4), (20, 2)]
            a1 = [Hv(114 + 8 * i, 8, 512) for i in range(3)]

            def ada1_hook(it):
                if it < 12:
                    ada_dma(1, it, a1[it % 3], B("a1", it % 3))
                if 2 <= it < 14:
                    ada_mm(1, it - 2, a1[(it - 2) % 3], B("a1", (it - 2) % 3), 7)

            ffn(0, groups,
                lambda gi: ffg[:, groups[gi][0] * 128:(groups[gi][0] + groups[gi][1]) * 128],
                lambda gi: ffu[:, groups[gi][0] * 128:(groups[gi][0] + groups[gi][1]) * 128],
                lambda gi: ffd[groups[gi][0] * 128:(groups[gi][0] + groups[gi][1]) * 128, :],
                hook=ada1_hook, dbanks=3)
            ada_add(1, 7)
            ada_derive(1)
            P.barrier()

        if upto >= 3:
            modulate(hsrc, hbuf, 512, NT, xdst, xbuf, lambda kc: PVC(1, 0, kc), lambda kc: PVC(1, 1, kc))
            uT = Hv(106, 8, S)
            ring = [Hv(138 + 8 * i, 8, 512) for i in range(4)]
            vtf = Fv(170, 1024)
            vsq = Fv(174, 1024)
            vtk = Hv(178, 2, 1024)
            gtb = Fv(182, 2, 1024)
            wsT = Hv(198, 512)
            bsr = Fv(199, 512)
            st_ = Fv(201, 8)
            stmp = Fv(202, 2, 128)
            P.dma("sp", gtb.rearrange("p a b -> p (a b)"), lnb, writes=[B("gtb")])
            P.dma("pool", wsT, wsTd, writes=[B("wsT")])
            P.dma("sp", bsr, bsd[0:1, :].partition_broadcast(128), writes=[B("bsr")])
            for s in range(4):
                slab_load(ring[s], sgi[:, s * 512:(s + 1) * 512], B("sring", s))
            sc_ = 0
            for m in range(8):
                for n in range(NT):
                    bank = sc_ % 2
                    sc_ += 1
                    mmg(PS(bank), B("ps", bank), [(ring[m // 4][:, kc, (m % 4) * 128:(m % 4 + 1) * 128], xdst(kc, n)) for kc in range(8)],
                        [B("sring", m // 4)] + [xbuf(kc, n) for kc in range(8)])
                    P.op("act", (I("activation", uT[:, m, n * 512:(n + 1) * 512], PS(bank), AF.Gelu_apprx_tanh)),
                         reads=[B("ps", bank)], writes=[B("uT", m, n)])
            import os as _os
            _vmode = int(_os.environ.get("SGU_V", "9"))
            for j in range(16 if _vmode > 0 else 0):
                n = j // 4
                for hf in range(2):
                    bank = 2 + hf
                    mmg(PS(bank), B("ps", bank), [(xn[:, kc, j * 128:(j + 1) * 128], ring[2 + hf][:, kc, :]) for kc in range(8)],
                        [B("sring", 2 + hf)] + [xbuf(kc, n) for kc in range(8)])
                    P.op("act", (I("activation", vtf[:, hf * 512:(hf + 1) * 512], PS(bank), AF.Gelu_apprx_tanh)),
                         reads=[B("ps", bank)], writes=[B("vtf", hf)])
                if _vmode < 2:
                    continue
                P.op("dve", I("reduce_sum", st_[:, 0:1], vtf, AX.X), reads=[B("vtf", 0), B("vtf", 1)], writes=[B("st0")])
                P.op("dve", I("tensor_scalar", st_[:, 1:2], st_[:, 0:1], -1.0 / 1024, None, ALU.mult), reads=[B("st0")], writes=[B("st1")])
                P.op("act", I("activation", vsq, vtf, AF.Square, bias=st_[:, 1:2]), reads=[B("vtf", 0), B("vtf", 1), B("st1")], writes=[B("vsq")])
                P.op("dve", I("reduce_sum", st_[:, 2:3], vsq, AX.X), reads=[B("vsq")], writes=[B("st2")])
                P.op("act", I("activation", st_[:, 3:4], st_[:, 2:3], AF.Sqrt, bias=eps, scale=1.0 / 1024), reads=[B("st2")], writes=[B("st3")])
                P.op("dve", I("reciprocal", st_[:, 4:5], st_[:, 3:4]), reads=[B("st3")], writes=[B("st4")])
                P.op("dve", I("tensor_tensor", st_[:, 5:6], st_[:, 1:2], st_[:, 4:5], ALU.mult), reads=[B("st4"), B("st1")], writes=[B("st5")])
                P.op("act", I("activation", vsq, vtf, AF.Identity, bias=st_[:, 5:6], scale=st_[:, 4:5]),
                     reads=[B("vtf", 0), B("vtf", 1), B("st5"), B("st4")], writes=[B("vsq")])
                P.op("dve", I("tensor_tensor", vsq, vsq, gtb[:, 0, :], ALU.mult), reads=[B("vsq"), B("gtb")], writes=[B("vsq")])
                vb = j % 2
                P.op("dve", (I("tensor_tensor", vtk[:, vb, :], vsq, gtb[:, 1, :], ALU.add)), reads=[B("vsq"), B("gtb")], writes=[B("vtk", vb)])
                if _vmode < 3:
                    continue
                for cc in range(8):
                    g = cc // 2
                    bank = 4 + (cc % 4)
                    oap = psum[:, bank, 0:128]
                    mmg(oap, B("ps", bank),
                        [(vtk[:, vb, cc * 128:(cc + 1) * 128], wsT[:, g * 128:(g + 1) * 128])],
                        [B("vtk", vb), B("wsT")])
                    P.op("dve", (I("tensor_tensor", stmp[:, cc % 2, :], oap, bsr[:, g * 128:(g + 1) * 128], ALU.add)),
                         reads=[B("ps", bank), B("bsr")], writes=[B("stmp", cc % 2)])
                    P.op("dve", (I("tensor_tensor", uT[:, cc, j * 128:(j + 1) * 128], uT[:, cc, j * 128:(j + 1) * 128], stmp[:, cc % 2, :], ALU.mult)),
                         reads=[B("stmp", cc % 2), B("uT", cc, n)], writes=[B("uT", cc, n)])
            for s in range(2):
                slab_load(ring[s], sgo[:, s * 512:(s + 1) * 512], B("sring", s))
            octr = 0
            for m in range(8):
                for n in range(NT):
                    bank = 6 + octr % 2
                    octr += 1
                    mmg(PS(bank), B("ps", bank), [(ring[m // 4][:, kc, (m % 4) * 128:(m % 4 + 1) * 128], uT[:, kc, n * 512:(n + 1) * 512]) for kc in range(8)],
                        [B("sring", m // 4)] + [B("uT", kc, n) for kc in range(8)])
                    P.op("dve", (I("scalar_tensor_tensor", hsrc(m, n), PS(bank), PVC(1, 2, m), hsrc(m, n), ALU.mult, ALU.add)),
                         reads=[B("ps", bank), hbuf(m, n)], writes=[hbuf(m, n)])
            P.barrier()

        if upto >= 4:
            CAP = 640
            NB = 5
            hf32 = Fv(106, 8, 512)
            rw = Fv(122, 8, 8)
            lg = Fv(96, 16, 8)
            m1 = Fv(96.5, 16)
            mk1 = Fv(96.75, 16, 8)
            l2 = Fv(97.25, 16, 8)
            m2 = Fv(97.75, 16)
            mk2 = Fv(98, 16, 8)
            g1 = Fv(98.5, 16)
            g2 = Fv(98.75, 16)
            dn = Fv(99, 16, 8)
            gT = Fv(138, 2048)
            sel = Fv(146, 1024)
            msk = Fv(151, 16, 8)
            tot = Fv(151.5, 16, 8)
            off = Fv(152, 16, 8)
            pos = Fv(152.5, 16, 8)
            posm = Fv(153, 16, 8)
            cntv = Fv(153.5, 8)
            maxc = Fv(153.625, 8)
            condi = arena[:, int(153.75 * 256):int(153.75 * 256) + 1].bitcast(mybir.dt.int32)
            tri = Fv(154, 128)
            ones32 = Fv(154.5, 128)
            iota = Fv(155, CAP)
            slotcol = Fv(157.5, 8)
            identb = Hv(157.625, 128)
            P.dma("sp", rw, rtr.rearrange("(kc p) e -> p kc e", p=128), writes=[B("rw")])
            P.dma("sp", sel[0:8, :], seld, writes=[B("sel")])
            P.dma("sp", tri, cst2[:, 0:128], writes=[B("tri")])
            P.dma("sp", iota, cst2[:, 128:128 + CAP], writes=[B("iota")])
            P.dma("sp", slotcol, cst2[:, 768:776], writes=[B("slotcol")])
            P.op("pool", I("memset", ones32, 1.0), writes=[B("ones32")])
            P.op("pool", I("tensor_copy", identb, ident), reads=[B("ident")], writes=[B("identb")])
            for n in range(NT):
                modulate(lambda kc, nn, n=n: hsrc(kc, n), lambda kc, nn, n=n: hbuf(kc, n), 512, 1,
                         lambda kc, nn, n=n: xdst(kc, n), lambda kc, nn, n=n: xbuf(kc, n),
                         lambda kc: PVC(1, 3, kc), lambda kc: PVC(1, 4, kc),
                         f32_fn=lambda kc, nn: hf32[:, kc, :], f32_buf=lambda kc, nn: B("hf32", kc))
                for jj in range(4):
                    j = n * 4 + jj
                    mmg(psum[:, 0, j * 8:(j + 1) * 8], B("ps", 0),
                        [(hf32[:, kc, jj * 128:(jj + 1) * 128], rw[:, kc, :]) for kc in range(8)],
                        [B("rw")] + [B("hf32", kc) for kc in range(8)])
            fl = lambda t: t.rearrange("p a b -> p (a b)")
            P.op("dve", I("tensor_copy", fl(lg), psum[:, 0, 0:128]), reads=[B("ps", 0)], writes=[B("lg")])
            P.op("dve", I("tensor_reduce", m1, lg, AX.X, ALU.max), reads=[B("lg")], writes=[B("m1")])
            P.op("dve", I("tensor_tensor", mk1, lg, m1.unsqueeze(2).to_broadcast([128, 16, 8]), ALU.is_equal), reads=[B("lg"), B("m1")], writes=[B("mk1")])
            P.op("dve", I("scalar_tensor_tensor", l2, mk1, -1.0e30, lg, ALU.mult, ALU.add), reads=[B("mk1"), B("lg")], writes=[B("l2")])
            P.op("dve", I("tensor_reduce", m2, l2, AX.X, ALU.max), reads=[B("l2")], writes=[B("m2")])
            P.op("dve", I("tensor_tensor", mk2, l2, m2.unsqueeze(2).to_broadcast([128, 16, 8]), ALU.is_equal), reads=[B("l2"), B("m2")], writes=[B("mk2")])
            P.op("dve", I("tensor_tensor", msk, mk1, mk2, ALU.add), reads=[B("mk1"), B("mk2")], writes=[B("msk")])
            P.op("dve", I("tensor_tensor", g2, m1, m2, ALU.subtract), reads=[B("m1"), B("m2")], writes=[B("g2")])
            P.op("act", I("activation", g1, g2, AF.Sigmoid), reads=[B("g2")], writes=[B("g1")])
            P.op("dve", I("tensor_scalar", g2, g1, -1.0, 1.0, ALU.mult, ALU.add), reads=[B("g1")], writes=[B("g2")])
            P.op("dve", I("tensor_tensor", mk1, mk1, g1.unsqueeze(2).to_broadcast([128, 16, 8]), ALU.mult), reads=[B("mk1"), B("g1"), B("msk")], writes=[B("mk1")])
            P.op("dve", I("tensor_tensor", mk2, mk2, g2.unsqueeze(2).to_broadcast([128, 16, 8]), ALU.mult), reads=[B("mk2"), B("g2"), B("msk")], writes=[B("mk2")])
            P.op("dve", I("tensor_tensor", dn, mk1, mk2, ALU.add), reads=[B("mk1"), B("mk2")], writes=[B("dn")])
            for j in range(16):
                bank = 1 + (j // 4) % 2
                mmg(psum[0:8, bank, (j % 4) * 128:(j % 4 + 1) * 128], B("ps", bank), [(dn[:, j, :], ident)], [B("dn"), B("ident")])
                if j % 4 == 3:
                    q4 = j // 4
                    P.op("act", (I("activation", gT[0:8, q4 * 512:(q4 + 1) * 512], psum[0:8, bank, :], AF.Copy)),
                         reads=[B("ps", bank)], writes=[B("gT")])
            mmg(psum[:, 3, 0:128], B("ps", 3), [(tri, fl(msk))], [B("tri"), B("msk")])
            mmg(psum[:, 4, 0:128], B("ps", 4), [(ones32, fl(msk))], [B("ones32"), B("msk")])
            P.op("dve", I("tensor_copy", fl(tot), psum[:, 4, 0:128]), reads=[B("ps", 4)], writes=[B("tot")])
            P.op("dve", I("memset", off[:, 0, :], 0.0), writes=[B("off")])
            for j in range(1, 16):
                P.op("dve", I("tensor_tensor", off[:, j, :], off[:, j - 1, :], tot[:, j - 1, :], ALU.add), reads=[B("off"), B("tot")], writes=[B("off")])
            P.op("dve", I("tensor_tensor", fl(pos), psum[:, 3, 0:128], fl(off), ALU.add), reads=[B("ps", 3), B("off")], writes=[B("pos")])
            P.op("dve", I("scalar_tensor_tensor", fl(posm), fl(pos), 1.0, fl(msk), ALU.add, ALU.mult), reads=[B("pos"), B("msk")], writes=[B("posm")])
            P.op("dve", I("tensor_scalar", fl(posm), fl(posm), -1.0, None, ALU.add), reads=[B("posm")], writes=[B("posm")])
            P.op("dve", I("tensor_tensor", cntv, off[:, 15, :], tot[:, 15, :], ALU.add), reads=[B("off"), B("tot")], writes=[B("cntv")])
            P.op("dve", I("tensor_reduce", maxc[:, 0:1], cntv, AX.X, ALU.max), reads=[B("cntv")], writes=[B("maxc")])
            P.op("dve", I("tensor_copy", condi, maxc[:, 0:1]), reads=[B("maxc")], writes=[B("condi")])
            import os as _o3
            NPASS = int(_o3.environ.get("NPASS", "4"))
            posp = Fv(151, NPASS, 128)
            cnti = arena[:, int(153.875 * 256):int(153.875 * 256) + 8].bitcast(mybir.dt.int32)
            P.op("dve", I("tensor_copy", cnti, cntv), reads=[B("cntv")], writes=[B("cnti")])
            for p_ in range(NPASS):
                P.op("dve", I("tensor_scalar", posp[:, p_, :], fl(posm), -float(CAP * p_), None, ALU.add), reads=[B("posm")], writes=[B("posp")])
            P.barrier()

            def moe_sparse():
                TILES = [(0, 512), (512, CAP - 512)]
                xg = Hv(64, 8, CAP)
                Sel = Hv(74, 16, CAP)
                ybf = Hv(74, NB, 1024)
                SelT = Hv(84, 2, NB, 512)
                x_tok = Hv(106, 16, 1024)
                sA = Fv(138, 2, 512)
                sB = Fv(142, 2, 128)
                gBt = Fv(143, 2, 512)
                dgG = Fv(147, 4, 128)
                dgP = Fv(149, 4, 128)
                ring = [Hv(158 + 4 * i, 8, 256) for i in range(6)]
                yacc = Fv(182, NB, 1024)
                actb = Hv(202, 2, 2, CAP)
                dnf = fl(dn)
                petok = lambda: ("pe", P.count["pe"])
                tctr = 0
                for j in range(16):
                    for q in range(2):
                        bank = tctr % 4
                        tctr += 1
                        for kk in range(4):
                            kc = q * 4 + kk
                            mmg(psum[:, bank, kk * 128:(kk + 1) * 128], B("ps", bank), [(xn[:, kc, j * 128:(j + 1) * 128], identb)], [])
                        if tctr % 2:
                            P.op("act", I("activation", x_tok[:, j, q * 512:(q + 1) * 512], PS(bank), AF.Copy), reads=[B("ps", bank)], writes=[B("xtok", j)])
                        else:
                            P.op("dve", I("tensor_copy", x_tok[:, j, q * 512:(q + 1) * 512], PS(bank)), reads=[B("ps", bank)], writes=[B("xtok", j)])
                r1_free = petok()
                rc = [0]
                ac = [0]
                fc = [0]
                dc = [0]
                gc = [0]
                NG = 14
                allg = [(e_, g_) for e_ in range(8) for g_ in range(NG)]

                def load_gu(idx):
                    e_, g_ = allg[idx]
                    f0 = g_ * 2
                    slots = []
                    for t in range(2):
                        si_ = rc[0] % 6
                        rc[0] += 1
                        src = (mg if t == 0 else mu)[e_, g_, :, :]
                        P.dma("pool", ring[si_], src.rearrange("p (kc n) -> p kc n", kc=8), writes=[B("ring", si_)], sembuf=B("grp", idx % 2))
                        slots.append(si_)
                    return slots

                def load_d(idx, slots):
                    e_, g_ = allg[idx]
                    f0 = g_ * 2
                    si_ = rc[0] % 6
                    rc[0] += 1
                    dv = ring[si_].rearrange("p a b -> p (a b)").rearrange("p (f n) -> p f n", n=1024)
                    tok = P.dma("pool", dv, md[e_, g_, :, :].rearrange("p (f n) -> p f n", f=2), writes=[B("ring", si_)], sembuf=B("grp", idx % 2))
                    slots = slots + [si_]
                    for s2 in slots:
                        B("ring", s2).w = tok
                    return slots

                def load_group(idx):
                    return load_d(idx, load_gu(idx))

                def emit_down(wd_, sd, ab, first, last, gather_tok):
                    if last:
                        P.wait_tok("dve", gather_tok)
                    for b in range(NB):
                        for half in range(2):
                            db = 4 + dc[0] % 4
                            dc[0] += 1
                            mmg(PS(db), B("ps", db), [(actb[:, ab, f, b * 128:(b + 1) * 128], wd_[:, f, half * 512:(half + 1) * 512]) for f in range(2)],
                                [B("ring", sd), B("act", ab)])
                            ya = yacc[:, b, half * 512:(half + 1) * 512]
                            if first:
                                P.op("dve", I("tensor_copy", ya, PS(db)), reads=[B("ps", db)], writes=[B("yacc", b, half)])
                            elif last:
                                P.op("dve", I("tensor_tensor", ybf[:, b, half * 512:(half + 1) * 512], ya, PS(db), ALU.add),
                                     reads=[B("ps", db), B("yacc", b, half)], writes=[B("ybf", b)])
                            else:
                                P.op("dve", I("tensor_tensor", ya, ya, PS(db), ALU.add), reads=[B("ps", db), B("yacc", b, half)], writes=[B("yacc", b, half)])

                st = {"r1": r1_free, "pend": None}

                def expert_block(e_, p_, chained):
                    posf = posp[:, p_, :]
                    if st["r1"] is not None:
                        P.wait_tok("pool", st["r1"])
                        P.wait_tok("dve", st["r1"])
                    for j in range(16):
                        eng = "dve"
                        P.op(eng, I("tensor_scalar", Sel[:, j, :], iota, posf[:, j * 8 + e_:j * 8 + e_ + 1], None, ALU.is_equal),
                             reads=[B("posp"), B("iota")], writes=[B("Sel", j)])
                    for kc in range(8):
                        for (o_, W) in TILES:
                            bank = gc[0] % 4
                            gc[0] += 1
                            jl = list(range(16)) if o_ == 0 else list(range(o_ // 128, 16))
                            mmg(PS(bank, W), B("ps", bank), [(x_tok[:, j, kc * 128:(kc + 1) * 128], Sel[:, j, o_:o_ + W]) for j in jl],
                                [B("xtok", j) for j in jl] + [B("Sel", j) for j in jl])
                            if gc[0] % 2:
                                P.op("act", I("activation", xg[:, kc, o_:o_ + W], PS(bank, W), AF.Copy), reads=[B("ps", bank)], writes=[B("xg", kc)])
                            else:
                                P.op("dve", I("tensor_copy", xg[:, kc, o_:o_ + W], PS(bank, W)), reads=[B("ps", bank)], writes=[B("xg", kc)])
                    gather_tok = petok()
                    pdown = None
                    pend = st["pend"] if (chained and st["pend"] is not None) else load_group(e_ * NG)
                    st["pend"] = None
                    for g_ in range(NG):
                        sg, su, sd = pend
                        nxt = None
                        if g_ + 1 < NG:
                            nxt = e_ * NG + g_ + 1
                        elif chained and e_ + 1 < 8:
                            nxt = (e_ + 1) * NG
                        ngu = load_gu(nxt) if nxt is not None else None
                        wg_, wu_ = ring[sg], ring[su]
                        wd_ = ring[sd].rearrange("p a b -> p (a b)").rearrange("p (f n) -> p f n", n=1024)
                        ab = ac[0] % 2
                        ac[0] += 1
                        for (o_, W) in TILES:
                            for f in range(2):
                                k = fc[0] % 2
                                fc[0] += 1
                                gb_, ub_ = k, 2 + k
                                xr = [B("xg", kc) for kc in range(8)]
                                mmg(PS(gb_, W), B("ps", gb_), [(wg_[:, kc, f * 128:(f + 1) * 128], xg[:, kc, o_:o_ + W]) for kc in range(8)], [B("ring", sg)] + xr)
                                mmg(PS(ub_, W), B("ps", ub_), [(wu_[:, kc, f * 128:(f + 1) * 128], xg[:, kc, o_:o_ + W]) for kc in range(8)], [B("ring", su)] + xr)
                                st_ = sA[:, k, :] if W == 512 else sB[:, k, 0:W]
                                sb_ = B("sA", k) if W == 512 else B("sB", k)
                                P.op("act", I("activation", st_, PS(gb_, W), AF.Silu), reads=[B("ps", gb_)], writes=[sb_])
                                P.op("dve", I("tensor_tensor", actb[:, ab, f, o_:o_ + W], st_, PS(ub_, W), ALU.mult),
                                     reads=[sb_, B("ps", ub_)], writes=[B("act", ab)])
                        if pdown is not None:
                            emit_down(*pdown)
                        pdown = (wd_, sd, ab, g_ == 0, g_ == NG - 1, gather_tok)
                        if nxt is not None:
                            full = load_d(nxt, ngu)
                            if g_ + 1 < NG:
                                pend = full
                            else:
                                st["pend"] = full
                    emit_down(*pdown)
                    for n in range(NT):
                        nb2 = n % 2
                        bx, by = (0, 1) if nb2 == 0 else (2, 3)
                        for jj in range(4):
                            j = n * 4 + jj
                            c_ = j * 8 + e_
                            P.op("dve", I("tensor_scalar", dgG[:, jj, :], ident, dnf[:, c_:c_ + 1], None, ALU.mult), reads=[B("dn"), B("ident")], writes=[B("dgG", jj)])
                            P.op("dve", I("tensor_scalar", dgP[:, jj, :], ident, posf[:, c_:c_ + 1], None, ALU.mult), reads=[B("posp"), B("ident")], writes=[B("dgP", jj)])
                            mmg(psum[:, bx, jj * 128:(jj + 1) * 128], B("ps", bx), [(ones32, dgG[:, jj, :])], [B("ones32"), B("dgG", jj)])
                            mmg(psum[:, by, jj * 128:(jj + 1) * 128], B("ps", by), [(ones32, dgP[:, jj, :])], [B("ones32"), B("dgP", jj)])
                        P.op("act", I("activation", gBt[:, nb2, :], PS(bx), AF.Copy), reads=[B("ps", bx)], writes=[B("gBt", nb2)])
                        bl = [b for b in range(NB) if 128 * b < (n + 1) * 512]
                        for b in bl:
                            P.op("dve", I("scalar_tensor_tensor", SelT[:, nb2, b, :], PS(by), slotcol[:, b:b + 1], gBt[:, nb2, :], ALU.is_equal, ALU.mult),
                                 reads=[B("ps", by), B("gBt", nb2), B("slotcol")], writes=[B("SelT", nb2, b)])
                        for m in range(8):
                            bank = 4 + m % 4
                            mmg(PS(bank), B("ps", bank), [(ybf[:, b, m * 128:(m + 1) * 128], SelT[:, nb2, b, :]) for b in bl],
                                [B("ybf", b) for b in bl] + [B("SelT", nb2, b) for b in bl])
                            P.op("dve", (I("scalar_tensor_tensor", hsrc(m, n), PS(bank), PVC(1, 5, m), hsrc(m, n), ALU.mult, ALU.add)),
                                 reads=[B("ps", bank), hbuf(m, n)], writes=[hbuf(m, n)])
                    st["r1"] = petok()


                for e_ in range(8):
                    expert_block(e_, 0, True)
                for p_ in range(1, NPASS):
                    for e_ in range(8):
                        P.barrier()
                        st["r1"] = None
                        st["pend"] = None
                        P.skip_begin(cnti[0:1, e_:e_ + 1], p_ * CAP + 1)
                        expert_block(e_, p_, False)
                        P.skip_end()

            def moe_dense():
                gB = Fv(114, 2048)
                groups = [(e_, f4 * 4, 4) for e_ in range(8) for f4 in range(7)]
                gstate = {"e": -1}

                def gate_fn(gi, n):
                    e_ = groups[gi][0]
                    if gstate["e"] != e_:
                        gstate["e"] = e_
                        for nn in range(NT):
                            mmg(PS(0), B("ps", 0), [(sel[0:8, e_ * 128:(e_ + 1) * 128], gT[0:8, nn * 512:(nn + 1) * 512])], [B("sel"), B("gT")])
                            P.op("act", (I("activation", gB[:, nn * 512:(nn + 1) * 512], PS(0), AF.Copy)),
                                 reads=[B("ps", 0)], writes=[B("gB", nn)])
                    return gB[:, n * 512:(n + 1) * 512], B("gB", n)

                ffn(1, [(g[1], g[2]) for g in groups],
                    lambda gi: mg[groups[gi][0], :, groups[gi][1] * 128:(groups[gi][1] + 4) * 128],
                    lambda gi: mu[groups[gi][0], :, groups[gi][1] * 128:(groups[gi][1] + 4) * 128],
                    lambda gi: md[groups[gi][0], groups[gi][1] * 128:(groups[gi][1] + 4) * 128, :],
                    gate_fn=gate_fn, ring_base=158, actb_base=122)

            moe_sparse()
            P.barrier()

        for kc in range(8):
            P.dma("sp", yT[kc * 128:(kc + 1) * 128, :], hT[:, kc, :], reads=[B("h", kc, n) for n in range(NT)], writes=[B("out", kc)], sembuf=B("ost", kc % 4))
        for tok in list(P.dma_toks.values()):
            P.wait_tok("sp", tok)
        P.emit()
    return nc


def _fm(v):
    v = np.asarray(v, np.float32)
    return np.ascontiguousarray(v.reshape(-1, 128).T)


def _consts():
    rows = S // 64
    row = np.repeat(np.arange(rows), 64).astype(np.float32)
    col = np.tile(np.arange(64), rows).astype(np.float32)
    inv = (10000.0 ** (-np.arange(16, dtype=np.float32) / 16)).astype(np.float32)
    ar = row[:, None] * inv
    ac = col[:, None] * inv
    ang = np.concatenate([ar, ar, ac, ac], axis=-1)
    cos = np.cos(ang).astype(np.float32).T
    sin = np.sin(ang).astype(np.float32).T
    rope = np.stack([np.concatenate([cos, cos], 0), np.concatenate([sin, sin], 0)], axis=1)
    bones = np.zeros((128, 128), np.float32)
    bones[:64, :64] = 1.0
    bones[64:, 64:] = 1.0
    prot = np.zeros((128, 128), np.float32)
    for base in (0, 64):
        for d in range(64):
            q = d // 16
            if q % 2 == 0:
                prot[base + d + 16, base + d] = -1.0
            else:
                prot[base + d - 16, base + d] = 1.0
    ident = np.eye(128, dtype=np.float32)
    mats = np.ascontiguousarray(np.stack([bones, prot, ident], axis=1))
    sel = np.zeros((8, 8, 128), np.float32)
    for e in range(8):
        sel[e, e, :] = 1.0
    cst2 = np.zeros((128, 776), np.float32)
    cst2[:, 0:128] = np.triu(np.ones((128, 128), np.float32), k=1)
    cst2[:, 128:768] = np.arange(640, dtype=np.float32)[None, :]
    cst2[:, 768:776] = (128.0 * np.arange(8, dtype=np.float32))[None, :] + np.arange(128, dtype=np.float32)[:, None]
    return np.ascontiguousarray(rope), mats, np.ascontiguousarray(sel.reshape(8, 1024)), cst2


_CACHE = {}


def kernel(x, c, ctx, c_ctx, ada_w, ada_b, norm_mix_g, norm_ffn_g,
           w_in_even, q_norm_g, k_norm_g, lam_q1, lam_k1, lam_q2, lam_k2, subln_g,
           conv_w, w_out_even, ffn_w_gate, ffn_w_up, ffn_w_down,
           sgu_w_in, sgu_ln_g, sgu_ln_b, sgu_w_s, sgu_b_s, sgu_w_out,
           router_w, moe_w_gate, moe_w_up, moe_w_down, _upto=4, _nb=None):
    f = lambda a: np.ascontiguousarray(np.asarray(a, dtype=np.float32))
    x = f(x)
    ctx = f(ctx)
    c = f(c)
    rope, mats, sel, cst2 = _consts()
    nb = x.shape[0] if _nb is None else _nb
    shared = {
        "lnb": np.ascontiguousarray(np.broadcast_to(np.concatenate([f(sgu_ln_g)[0], f(sgu_ln_b)[0]])[None, :], (128, 2048))),
        "bs": f(sgu_b_s)[0].reshape(1, 512),
        "rope": rope, "mats": mats, "sel": sel, "cst2": cst2,
        "ada_w": f(ada_w), "w_in": f(w_in_even)[0], "w_out": f(w_out_even)[0],
        "ffn_g": f(ffn_w_gate)[0], "ffn_u": f(ffn_w_up)[0], "ffn_d": f(ffn_w_down)[0],
        "sgu_in": f(sgu_w_in)[0],
        "wsT": np.ascontiguousarray(np.transpose(f(sgu_w_s)[0], (2, 0, 1)).reshape(128, 512)),
        "sgu_out": f(sgu_w_out)[0], "router": f(router_w)[0],
        "moe_g": np.ascontiguousarray(f(moe_w_gate)[0].reshape(8, 8, 128, 14, 256).transpose(0, 3, 2, 1, 4)).reshape(8, 14, 128, 2048),
        "moe_u": np.ascontiguousarray(f(moe_w_up)[0].reshape(8, 8, 128, 14, 256).transpose(0, 3, 2, 1, 4)).reshape(8, 14, 128, 2048),
        "moe_d": np.ascontiguousarray(f(moe_w_down)[0].reshape(8, 14, 2, 128, 1024).transpose(0, 1, 3, 2, 4)).reshape(8, 14, 128, 2048),
    }
    vbase = np.zeros((128, NV), np.float32)
    vbase[:, 8:16] = _fm(c_ctx)
    vbase[:, 16:64] = _fm(f(ada_b)[0])
    vbase[:, 64:112] = _fm(f(ada_b)[1])
    vbase[:, 112:120] = _fm(f(norm_mix_g)[0])
    vbase[:, 120:128] = _fm(f(norm_mix_g)[1])
    vbase[:, 128:136] = _fm(f(norm_ffn_g)[0])
    vbase[:, 136:144] = _fm(f(norm_ffn_g)[1])
    vbase[:, 144] = np.tile(f(q_norm_g)[0], 2)
    vbase[:, 145] = np.tile(f(k_norm_g)[0], 2)
    vbase[:, 146] = f(subln_g)[0]
    cw = f(conv_w)[0]
    for k in range(3):
        vbase[:, 147 + 4 * k:151 + 4 * k] = _fm(cw[k])
    vbase[:, 159] = 1e-6
    for i, lv in enumerate((lam_q1, lam_k1, lam_q2, lam_k2)):
        vbase[:, 160 + 64 * i:224 + 64 * i] = np.broadcast_to(f(lv)[0][None, :], (128, 64))
    in_maps = []
    for b in range(nb):
        v = vbase.copy()
        v[:, 0:8] = _fm(c[b])
        m = dict(shared)
        m["xT"] = np.ascontiguousarray(x[b].T)
        m["ctxT"] = np.ascontiguousarray(ctx[b].T)
        m["vecs"] = v
        in_maps.append(m)
    key = _upto
    if key not in _CACHE:
        _CACHE[key] = build_program(_upto)
    nc = _CACHE[key]
    res = run_bass_kernel_spmd(nc, in_maps, core_ids=list(range(nb)))
    out = np.stack([np.ascontiguousarray(r["yT"].T) for r in res.results], axis=0)
    return out.astype(np.float32)
```
